# Optimizing a Trainium2 kernel written in Bass

```python
import math
import jax
import jax.numpy as jnp
from jax import lax
import numpy as np

D_MODEL = 1024
BATCH = 2
SEQ = 16384
DEPTH = 2

H_A = 8
DK_A = 64
DV_A = 64
CONV_K = 4
CHUNK = 64
H_B = 8
H_B_KV = 2
DH_B = 64
WINDOW = 128
ATT_BLK = 128
NUM_BUCKETS = 32
MAX_DISTANCE = 128
POOL_WINDOWS = (2, 4, 8, 16)
N_POOL_GROUPS = 4
POOL_GROUP_DIM = D_MODEL // N_POOL_GROUPS
N_GROUPS = 4
EXPERTS_PER_GROUP = 8
N_EXPERTS = N_GROUPS * EXPERTS_PER_GROUP
TOP_K_INNER = 2
D_FF_EXPERT = D_MODEL // 2
MOE_BLK = 128
EPS = 1e-6

WA = H_A * DK_A
VA = H_A * DV_A
WB = H_B * DH_B
WKV_B = H_B_KV * DH_B
IN_SIZES = (WA, WA, VA, VA, H_A, H_A, WB, WKV_B, WKV_B)
P_IN = 2 * WA + 2 * VA + 2 * H_A + WB + 2 * WKV_B

kernel_name = 'hybrid_deltanet_swa_pool_hmoe'


def rms_norm(x, w):
    x32 = x.astype(jnp.float32)
    y = x32 * lax.rsqrt(jnp.mean(x32 * x32, axis=-1, keepdims=True) + EPS)
    return (y * w.astype(jnp.float32)).astype(x.dtype)


def l2_normalize(x):
    return x * lax.rsqrt(jnp.sum(x * x, axis=-1, keepdims=True) + EPS)


def causal_depthwise_conv(x, w):
    ch = x.shape[-1]
    return lax.conv_general_dilated(x, w[:, None, :].astype(x.dtype), window_strides=(1,), padding=((CONV_K - 1, 0),), dimension_numbers=('NWC', 'WIO', 'NWC'), feature_group_count=ch)


def t5_causal_bucket(dist):
    n = jnp.maximum(dist, 0)
    max_exact = NUM_BUCKETS // 2
    nf = jnp.maximum(n, 1).astype(jnp.float32)
    large = max_exact + (jnp.log(nf / max_exact) / math.log(MAX_DISTANCE / max_exact) * (NUM_BUCKETS - max_exact)).astype(jnp.int32)
    large = jnp.minimum(large, NUM_BUCKETS - 1)
    return jnp.where(n < max_exact, n, large)


def chunk_gated_delta_rule(q, k, v, g, beta):
    bsz, nh, seq, dk = q.shape
    dv = v.shape[-1]
    n = seq // CHUNK
    q = q.reshape(bsz, nh, n, CHUNK, dk)
    k = k.reshape(bsz, nh, n, CHUNK, dk)
    v = v.reshape(bsz, nh, n, CHUNK, dv)
    g = g.reshape(bsz, nh, n, CHUNK)
    beta = beta.reshape(bsz, nh, n, CHUNK)
    gc = jnp.cumsum(g, axis=-1)
    incl = jnp.tril(jnp.ones((CHUNK, CHUNK), dtype=bool))
    strict = jnp.tril(jnp.ones((CHUNK, CHUNK), dtype=bool), -1)
    decay = jnp.where(incl, jnp.exp(jnp.where(incl, gc[..., :, None] - gc[..., None, :], 0.0)), 0.0)
    kb = k * beta[..., None]
    a_mat = jnp.eye(CHUNK, dtype=jnp.float32) + jnp.where(strict, jnp.einsum('bhnid,bhnjd->bhnij', kb, k) * decay, 0.0)
    rhs = jnp.concatenate([v * beta[..., None], kb * jnp.exp(gc)[..., None]], axis=-1)
    sol = lax.linalg.triangular_solve(a_mat, rhs, left_side=True, lower=True, unit_diagonal=True)
    u, w = sol[..., :dv], sol[..., dv:]
    a_qk = jnp.einsum('bhnid,bhnjd->bhnij', q, k) * decay
    q_dec = q * jnp.exp(gc)[..., None]
    k_dec = k * jnp.exp(gc[..., -1:] - gc)[..., None]
    g_tot = jnp.exp(gc[..., -1])

    def step(state, inp):
        a_n, qd, kd, u_n, w_n, gt = inp
        v_new = u_n - jnp.einsum('bhck,bhkv->bhcv', w_n, state)
        o = jnp.einsum('bhck,bhkv->bhcv', qd, state) + jnp.einsum('bhij,bhjv->bhiv', a_n, v_new)
        state = state * gt[..., None, None] + jnp.einsum('bhck,bhcv->bhkv', kd, v_new)
        return state, o

    xs = (jnp.moveaxis(a_qk, 2, 0), jnp.moveaxis(q_dec, 2, 0), jnp.moveaxis(k_dec, 2, 0), jnp.moveaxis(u, 2, 0), jnp.moveaxis(w, 2, 0), jnp.moveaxis(g_tot, 2, 0))
    s0 = jnp.zeros((bsz, nh, dk, dv), jnp.float32)
    _, o = lax.scan(step, s0, xs)
    return jnp.moveaxis(o, 0, 2).reshape(bsz, nh, seq, dv)


def sliding_window_sink_attention(q, k, v, sinks, rel_bias):
    bsz, seq, _ = q.shape
    nb = seq // ATT_BLK
    grp = H_B // H_B_KV
    qb = q.reshape(bsz, nb, ATT_BLK, H_B_KV, grp, DH_B).astype(jnp.float32) * (DH_B ** -0.5)

    def band(t):
        t = t.reshape(bsz, seq, H_B_KV, DH_B)
        tp = jnp.concatenate([jnp.zeros_like(t[:, :ATT_BLK]), t], axis=1).reshape(bsz, nb + 1, ATT_BLK, H_B_KV, DH_B)
        return jnp.concatenate([tp[:, :-1], tp[:, 1:]], axis=2).astype(jnp.float32)

    kw = band(k)
    vw = band(v)
    s = jnp.einsum('bnqhgd,bnkhd->bhgnqk', qb, kw)
    qi = jnp.arange(ATT_BLK)[:, None]
    ki = jnp.arange(2 * ATT_BLK)[None, :]
    dist = qi + ATT_BLK - ki
    bias = rel_bias.astype(jnp.float32)[t5_causal_bucket(dist)]
    bias = bias.transpose(2, 0, 1).reshape(H_B_KV, grp, 1, ATT_BLK, 2 * ATT_BLK)
    key_pos = jnp.arange(nb)[:, None, None] * ATT_BLK + ki[None] - ATT_BLK
    valid = (dist >= 0) & (dist < WINDOW) & (key_pos >= 0)
    s = jnp.where(valid, s + bias, -1e30)
    sink = sinks.astype(jnp.float32).reshape(H_B_KV, grp, 1, 1, 1)
    m = jnp.maximum(jnp.max(s, axis=-1, keepdims=True), sink)
    p = jnp.exp(s - m)
    denom = jnp.sum(p, axis=-1, keepdims=True) + jnp.exp(sink - m)
    o = jnp.einsum('bhgnqk,bnkhd->bnqhgd', p / denom, vw)
    return o.reshape(bsz, seq, WB).astype(q.dtype)


def deltanet_swa_mixer(h, w_in, conv_w, a_log, dt_bias, onorm_w, sinks, rel_bias, w_out):
    bsz, seq, _ = h.shape
    f32 = jnp.float32
    proj = h @ w_in
    cuts = np.cumsum(IN_SIZES)[:-1].tolist()
    q_a, k_a, v_a, z_a, b_a, a_a, q_b, k_b, v_b = jnp.split(proj, cuts, axis=-1)
    qkv = jax.nn.silu(causal_depthwise_conv(jnp.concatenate([q_a, k_a, v_a], axis=-1), conv_w))
    qa, ka, va = jnp.split(qkv, [WA, 2 * WA], axis=-1)
    qa = l2_normalize(qa.reshape(bsz, seq, H_A, DK_A).transpose(0, 2, 1, 3).astype(f32)) * (DK_A ** -0.5)
    ka = l2_normalize(ka.reshape(bsz, seq, H_A, DK_A).transpose(0, 2, 1, 3).astype(f32))
    va = va.reshape(bsz, seq, H_A, DV_A).transpose(0, 2, 1, 3).astype(f32)
    beta = jax.nn.sigmoid(b_a.astype(f32)).transpose(0, 2, 1)
    g = (-jnp.exp(a_log.astype(f32)) * jax.nn.softplus(a_a.astype(f32) + dt_bias.astype(f32))).transpose(0, 2, 1)
    o_a = chunk_gated_delta_rule(qa, ka, va, g, beta).transpose(0, 2, 1, 3)
    o_a = rms_norm(o_a, onorm_w) * jax.nn.silu(z_a.reshape(bsz, seq, H_A, DV_A).astype(f32))
    out_a = o_a.reshape(bsz, seq, VA).astype(h.dtype)
    out_b = sliding_window_sink_attention(q_b, k_b, v_b, sinks, rel_bias)
    return jnp.concatenate([out_a, out_b], axis=-1) @ w_out


def multiscale_pool_mixer(h, pool_w, pool_scale):
    bsz, seq, d = h.shape
    f32 = jnp.float32
    hg = h.astype(f32).reshape(bsz, seq, N_POOL_GROUPS, POOL_GROUP_DIM)
    cs = jnp.cumsum(hg, axis=1)
    t = jnp.arange(seq)
    pooled = []
    for gi, win in enumerate(POOL_WINDOWS):
        hi = cs[:, :, gi]
        lo = jnp.concatenate([jnp.zeros((bsz, win, POOL_GROUP_DIM), f32), cs[:, :seq - win, gi]], axis=1)
        count = jnp.minimum(t + 1, win).astype(f32)[None, :, None]
        pooled.append((hi - lo) / count - hg[:, :, gi])
    pooled = jnp.stack(pooled, axis=2).astype(h.dtype)
    y = jnp.einsum('blgc,gcd->blgd', pooled, pool_w).reshape(bsz, seq, d)
    return y * pool_scale


def hierarchical_moe(h, r1_w, r1_b, r2_w, r2_b, w_gate, w_up, w_down):
    bsz, seq, d = h.shape
    n_tok = bsz * seq
    xf = h.reshape(n_tok, d)
    p_grp = jax.nn.softmax((xf @ r1_w).astype(jnp.float32) + r1_b.astype(jnp.float32), axis=-1)
    p_top, grp = lax.top_k(p_grp, 1)
    p_top = p_top[:, 0]
    grp = grp[:, 0]
    lg2 = ((xf @ r2_w).astype(jnp.float32) + r2_b.astype(jnp.float32)).reshape(n_tok, N_GROUPS, EXPERTS_PER_GROUP)
    lg2 = lg2[jnp.arange(n_tok), grp]
    top_l, top_e = lax.top_k(lg2, TOP_K_INNER)
    gate = p_top[:, None] * jax.nn.softmax(top_l, axis=-1)
    expert = grp[:, None] * EXPERTS_PER_GROUP + top_e
    n_asg = n_tok * TOP_K_INNER
    e_flat = expert.reshape(n_asg)
    g_flat = gate.reshape(n_asg)
    tok_flat = jnp.arange(n_asg, dtype=jnp.int32) // TOP_K_INNER
    order = jnp.argsort(e_flat)
    e_s = e_flat[order]
    g_s = g_flat[order]
    tok_s = tok_flat[order]
    counts = jnp.zeros((N_EXPERTS,), jnp.int32).at[e_flat].add(1)
    starts = jnp.cumsum(counts) - counts
    padded = (counts + MOE_BLK - 1) // MOE_BLK * MOE_BLK
    pad_end = jnp.cumsum(padded)
    pad_start = pad_end - padded
    dest = pad_start[e_s] + (jnp.arange(n_asg, dtype=jnp.int32) - starts[e_s])
    n_rows = (n_asg + N_EXPERTS * (MOE_BLK - 1) + MOE_BLK - 1) // MOE_BLK * MOE_BLK
    n_blk = n_rows // MOE_BLK
    row_tok = jnp.full((n_rows,), n_tok, jnp.int32).at[dest].set(tok_s)
    row_gate = jnp.zeros((n_rows,), jnp.float32).at[dest].set(g_s)
    blk_expert = jnp.minimum(jnp.searchsorted(pad_end, jnp.arange(n_blk, dtype=jnp.int32) * MOE_BLK, side='right'), N_EXPERTS - 1)
    x_pad = jnp.concatenate([xf, jnp.zeros((1, d), xf.dtype)], axis=0)
    x_rows = x_pad[row_tok].reshape(n_blk, MOE_BLK, d)

    def expert_block(args):
        xb, e = args
        return (jax.nn.silu(xb @ w_gate[e]) * (xb @ w_up[e])) @ w_down[e]

    y_rows = lax.map(expert_block, (x_rows, blk_expert)).reshape(n_rows, d)
    y = jax.ops.segment_sum(y_rows * row_gate[:, None].astype(y_rows.dtype), row_tok, num_segments=n_tok + 1)[:n_tok]
    return y.reshape(bsz, seq, d)


def setup_inputs(seed: int = 0) -> dict:
    key = jax.random.key(seed)
    ks = jax.random.split(key, 24)
    f32 = jnp.float32
    d = D_MODEL
    n_even = (DEPTH + 1) // 2
    n_odd = DEPTH // 2

    def nrm(k, shape, scale):
        return jax.random.normal(k, shape, f32) * scale

    x = nrm(ks[0], (BATCH, SEQ, d), 1.0)
    c = nrm(ks[1], (BATCH, d), 1.0)
    ada_w = nrm(ks[2], (DEPTH, d, 6 * d), 0.5 * d ** -0.5)
    ada_b = nrm(ks[3], (DEPTH, 6 * d), 0.02)
    norm_mix_w = 1.0 + nrm(ks[4], (DEPTH, d), 0.02)
    norm_ffn_w = 1.0 + nrm(ks[5], (DEPTH, d), 0.02)
    ab_w_in = nrm(ks[6], (n_even, d, P_IN), d ** -0.5)
    ab_conv_w = nrm(ks[7], (n_even, CONV_K, 3 * WA), CONV_K ** -0.5)
    ab_a_log = jnp.log(jax.random.uniform(ks[8], (n_even, H_A), f32, 1.0, 16.0))
    dt0 = jnp.exp(jax.random.uniform(ks[9], (n_even, H_A), f32, math.log(1e-3), math.log(1e-1)))
    ab_dt_bias = dt0 + jnp.log(-jnp.expm1(-dt0))
    ab_onorm_w = 1.0 + nrm(ks[10], (n_even, DV_A), 0.02)
    ab_sinks = nrm(ks[11], (n_even, H_B), 0.5)
    rel_bias = nrm(ks[12], (NUM_BUCKETS, H_B), 0.5)
    ab_w_out = nrm(ks[13], (n_even, VA + WB, d), (VA + WB) ** -0.5)
    pool_w = nrm(ks[14], (n_odd, N_POOL_GROUPS, POOL_GROUP_DIM, POOL_GROUP_DIM), POOL_GROUP_DIM ** -0.5)
    pool_scale = 1.0 + nrm(ks[15], (n_odd, d), 0.1)
    r1_w = nrm(ks[16], (DEPTH, d, N_GROUPS), d ** -0.5)
    r1_b = nrm(ks[17], (DEPTH, N_GROUPS), 0.01)
    r2_w = nrm(ks[18], (DEPTH, d, N_EXPERTS), d ** -0.5)
    r2_b = nrm(ks[19], (DEPTH, N_EXPERTS), 0.01)
    moe_w_gate = nrm(ks[20], (DEPTH, N_EXPERTS, d, D_FF_EXPERT), d ** -0.5)
    moe_w_up = nrm(ks[21], (DEPTH, N_EXPERTS, d, D_FF_EXPERT), d ** -0.5)
    moe_w_down = nrm(ks[22], (DEPTH, N_EXPERTS, D_FF_EXPERT, d), D_FF_EXPERT ** -0.5)
    final_norm_w = 1.0 + nrm(ks[23], (d,), 0.02)
    return {'x': x, 'c': c, 'ada_w': ada_w, 'ada_b': ada_b, 'norm_mix_w': norm_mix_w, 'norm_ffn_w': norm_ffn_w, 'ab_w_in': ab_w_in, 'ab_conv_w': ab_conv_w, 'ab_a_log': ab_a_log, 'ab_dt_bias': ab_dt_bias, 'ab_onorm_w': ab_onorm_w, 'ab_sinks': ab_sinks, 'rel_bias': rel_bias, 'ab_w_out': ab_w_out, 'pool_w': pool_w, 'pool_scale': pool_scale, 'r1_w': r1_w, 'r1_b': r1_b, 'r2_w': r2_w, 'r2_b': r2_b, 'moe_w_gate': moe_w_gate, 'moe_w_up': moe_w_up, 'moe_w_down': moe_w_down, 'final_norm_w': final_norm_w}


def reference(x, c, ada_w, ada_b, norm_mix_w, norm_ffn_w, ab_w_in, ab_conv_w, ab_a_log, ab_dt_bias, ab_onorm_w, ab_sinks, rel_bias, ab_w_out, pool_w, pool_scale, r1_w, r1_b, r2_w, r2_b, moe_w_gate, moe_w_up, moe_w_down, final_norm_w):
    cond = jax.nn.silu(c)
    for layer in range(DEPTH):
        mod = cond @ ada_w[layer] + ada_b[layer]
        sh1, sc1, g1, sh2, sc2, g2 = [m[:, None, :] for m in jnp.split(mod, 6, axis=-1)]
        h = rms_norm(x, norm_mix_w[layer]) * (1 + sc1) + sh1
        i = layer // 2
        if layer % 2 == 0:
            y = deltanet_swa_mixer(h, ab_w_in[i], ab_conv_w[i], ab_a_log[i], ab_dt_bias[i], ab_onorm_w[i], ab_sinks[i], rel_bias, ab_w_out[i])
        else:
            y = multiscale_pool_mixer(h, pool_w[i], pool_scale[i])
        x = x + g1 * y
        h = rms_norm(x, norm_ffn_w[layer]) * (1 + sc2) + sh2
        x = x + g2 * hierarchical_moe(h, r1_w[layer], r1_b[layer], r2_w[layer], r2_b[layer], moe_w_gate[layer], moe_w_up[layer], moe_w_down[layer])
    return rms_norm(x, final_norm_w)
```

```python
import numpy as np
import concourse.bass as bass
import concourse.mybir as mybir
from concourse.bass_utils import run_bass_kernel_spmd

F32 = mybir.dt.float32
BF16 = mybir.dt.bfloat16
I32 = mybir.dt.int32
AF = mybir.ActivationFunctionType
ALU = mybir.AluOpType
AX = mybir.AxisListType


class P:
    ENG = ('pe', 'act', 'dve', 'pool', 'sp')

    def __init__(self, nc):
        self.nc = nc
        self.ops = {e: [] for e in self.ENG}
        self.cnt = {e: 0 for e in self.ENG}
        self.csem = {e: nc.alloc_semaphore("c_" + e) for e in self.ENG}
        self.dsem = {}
        self.dcnt = {}
        self.last_w = {}
        self.readers = {}
        self.known = {e: {} for e in self.ENG}
        self.out_tokens = []
        self.n_sb = 0

    def sb(self, name, shape, dtype):
        return self.nc.alloc_sbuf_tensor("s_" + name, list(shape), dtype)

    def ps(self, name, shape, dtype=F32):
        return self.nc.alloc_psum_tensor(name, list(shape), dtype)

    def _deps(self, reads, writes):
        toks = []
        for k in reads:
            t = self.last_w.get(k)
            if t is not None:
                toks.append(t)
        for k in writes:
            t = self.last_w.get(k)
            if t is not None:
                toks.append(t)
            toks.extend(self.readers.get(k, ()))
        return toks

    def _waits(self, eng, toks):
        best = {}
        for (sem, val, peng) in toks:
            if peng == eng and eng in ('pe', 'sp'):
                continue
            key = id(sem)
            if val <= self.known[eng].get(key, 0):
                continue
            if key not in best or best[key][1] < val:
                best[key] = (sem, val)
        for key, (sem, val) in best.items():
            self.known[eng][key] = val
        return list(best.values())

    def _commit(self, tok, reads, writes):
        for k in writes:
            self.last_w[k] = tok
            self.readers[k] = []
        for k in reads:
            if k in writes:
                continue
            self.readers.setdefault(k, []).append(tok)

    def op(self, eng, fn, reads=(), writes=()):
        waits = self._waits(eng, self._deps(reads, writes))
        self.cnt[eng] += 1
        tok = (self.csem[eng], self.cnt[eng], eng)
        self.ops[eng].append((waits, fn, (self.csem[eng], 1)))
        self._commit(tok, reads, writes)
        return tok

    def dma(self, q, out_ap, in_ap, reads=(), writes=(), out=False, sem=None, **kw):
        waits = self._waits(q, self._deps(reads, writes))
        skey = sem if sem is not None else writes[0]
        if skey not in self.dsem:
            self.dsem[skey] = self.nc.alloc_semaphore("d_%d" % len(self.dsem))
            self.dcnt[skey] = 0
        self.dcnt[skey] += 16
        s = self.dsem[skey]
        tok = (s, self.dcnt[skey], None)
        self.ops[q].append((waits, lambda e: e.dma_start(out_ap, in_ap, **kw), (s, 16)))
        self._commit(tok, reads, writes)
        if out:
            self.out_tokens.append(tok)
        return tok

    def barrier(self):
        toks = [(self.csem[e], self.cnt[e], None) for e in self.ENG if self.cnt[e] > 0]
        toks += [(self.dsem[k], self.dcnt[k], None) for k in self.dsem]
        for e in self.ENG:
            w = self._waits(e, toks)
            if w:
                self.ops[e].append((w, None, None))

    def emit(self):
        nc = self.nc
        fin = self._waits('sp', self.out_tokens)
        ops = self.ops

        def replay(eng_name, e):
            for waits, fn, inc in ops[eng_name]:
                for (sem, val) in waits:
                    e.wait_ge(sem, val)
                if fn is None:
                    continue
                ins = fn(e)
                ins.then_inc(inc[0], inc[1])

        with nc.Block() as block:
            @block.tensor
            def _(e):
                replay('pe', e)

            @block.scalar
            def _(e):
                replay('act', e)

            @block.vector
            def _(e):
                replay('dve', e)

            @block.gpsimd
            def _(e):
                replay('pool', e)

            @block.sync
            def _(e):
                replay('sp', e)
                for (sem, val) in fin:
                    e.wait_ge(sem, val)


D = 1024
NCH = 8
P_IN = 2832
EPS = 1e-6
NEG = -30000.0


class Cfg:
    def __init__(self, seq):
        self.SEQ = seq
        self.NBO = seq // 128 // 4
        self.NBE = self.NBO + 1
        self.NBT = 4 * self.NBO
        self.NT = self.NBT * 128
        self.NOWN = self.NBO * 128
        self.NEXT = self.NBE * 128
        self.PRE = self.NBT - self.NBE
        assert self.NBT % 4 == 0


def _names(aps):
    out = []
    for a in aps:
        if a is None or isinstance(a, (int, float)):
            continue
        n = a.name
        if n not in out:
            out.append(n)
    return out


import os
USE_POOL = os.environ.get("MK_POOL", "0") == "1"


def _e(eng):
    if eng == 'pool' and not USE_POOL:
        return 'dve'
    return eng


class Emit:
    def __init__(self, p):
        self.p = p

    def mm(self, out, lhsT, rhs, start=True, stop=True):
        self.p.op('pe', lambda e: e.matmul(out, lhsT, rhs, start=start, stop=stop),
                  reads=_names([lhsT, rhs]), writes=_names([out]))

    def tr(self, out, in_, ident):
        self.p.op('pe', lambda e: e.transpose(out, in_, ident),
                  reads=_names([in_, ident]), writes=_names([out]))

    def act(self, out, in_, func, bias=None, scale=None, accum=None, eng='act'):
        kw = {}
        if bias is not None:
            kw['bias'] = bias
        if scale is not None:
            kw['scale'] = scale
        if accum is not None:
            kw['accum_out'] = accum
        self.p.op('act', lambda e: e.activation(out, in_, func, **kw),
                  reads=_names([in_, bias, scale]), writes=_names([out, accum]))

    def tt(self, out, in0, in1, op, eng='dve'):
        eng = _e(eng)
        self.p.op(eng, lambda e: e.tensor_tensor(out, in0, in1, op),
                  reads=_names([in0, in1]), writes=_names([out]))

    def ts(self, out, in0, s1, op0, s2=None, op1=None, eng='dve'):
        eng = _e(eng)
        if op1 is None:
            self.p.op(eng, lambda e: e.tensor_scalar(out, in0, s1, None, op0),
                      reads=_names([in0, s1]), writes=_names([out]))
        else:
            self.p.op(eng, lambda e: e.tensor_scalar(out, in0, s1, s2, op0, op1),
                      reads=_names([in0, s1, s2]), writes=_names([out]))

    def stt(self, out, in0, scalar, in1, op0, op1, eng='dve'):
        eng = _e(eng)
        self.p.op(eng, lambda e: e.scalar_tensor_tensor(out, in0, scalar, in1, op0, op1),
                  reads=_names([in0, scalar, in1]), writes=_names([out]))

    def copy(self, out, in_, eng='act'):
        eng = _e(eng)
        if eng == 'act':
            self.p.op('act', lambda e: e.copy(out, in_), reads=_names([in_]), writes=_names([out]))
        else:
            self.p.op(eng, lambda e: e.tensor_copy(out, in_), reads=_names([in_]), writes=_names([out]))

    def red(self, out, in_, op, axis=AX.X):
        self.p.op('dve', lambda e: e.tensor_reduce(out, in_, axis, op),
                  reads=_names([in_]), writes=_names([out]))

    def recip(self, out, in_):
        self.p.op('dve', lambda e: e.reciprocal(out, in_), reads=_names([in_]), writes=_names([out]))

    def memset(self, ap, val, eng='dve'):
        eng = _e(eng)
        self.p.op(eng, lambda e: e.memset(ap, val), reads=[], writes=_names([ap]))

    def dma(self, out, in_, q='sp', final=False):
        self.p.dma(q, out, in_, reads=_names([in_]), writes=_names([out]), out=final)


MUL, ADD, SUB, MAX = ALU.mult, ALU.add, ALU.subtract, ALU.max


def build(cfg, stop_after=None, dbg=()):
    nc = bass.Bass("TRN2", target_bir_lowering=False)
    p = P(nc)
    E = Emit(p)
    NT, NEXT, NOWN = cfg.NT, cfg.NEXT, cfg.NOWN

    in_names = []

    def din(name, shape):
        in_names.append(name)
        return nc.dram_tensor(name, list(shape), F32, kind="ExternalInput").ap()

    xsT = din("xsT", [D, NT])
    maskrow = din("maskrow", [1, NT])
    condT_d = din("condT", [128, 8])
    ada_w = din("ada_w", [2, D, 6 * D])
    ada_b2 = din("ada_b2", [128, 2, 48])
    nw_d = din("nw", [128, 5, 8])
    w_in = din("w_in", [D, P_IN])
    w_out = din("w_out", [D, D])
    convw_d = din("convw", [128, 12, 4])
    rows_d = din("rows", [128, 160])
    bias_d = din("bias_tab", [128, 8, 256])
    cmask_d = din("cmask", [128, 12])
    poolfix_d = din("poolfix", [128, 8, 16])
    pool_w = din("pool_w", [4, 256, 256])
    pscale_d = din("pscale", [128, 8])
    r1w = r2w = wgate = wup = wdown = None
    if stop_after not in ('prologue', 'mixer0'):
        r1w = din("r1w", [2, D, 4])
        r2w = din("r2w", [2, D, 32])
        wgate = din("moe_w_gate", [2, 32, D, 512])
        wup = din("moe_w_up", [2, 32, D, 512])
        wdown = din("moe_w_down", [2, 32, 512, D])
    consts_d = din("consts", [128, 5, 128])
    outT = nc.dram_tensor("outT", [D, NOWN], F32, kind="ExternalOutput").ap()
    dbg_kind = "ExternalOutput" if dbg else "Internal"
    x1T = nc.dram_tensor("x1T", [D, NEXT], F32, kind=dbg_kind).ap()
    x2T = nc.dram_tensor("x2T", [D, NEXT], F32, kind=dbg_kind).ap()
    x3T = nc.dram_tensor("x3T", [D, NOWN], F32, kind=dbg_kind).ap()
    dbg_cat = nc.dram_tensor("dbg_cat", [NEXT, D], F32, kind="ExternalOutput").ap() if dbg else None

    psA = p.ps("psA", [128, 512]); psB = p.ps("psB", [128, 512]); psC = p.ps("psC", [128, 512])
    psD = p.ps("psD", [128, 512]); psE = p.ps("psE", [128, 512]); psF = p.ps("psF", [128, 512])
    psG = p.ps("psG", [128, 512]); psT = p.ps("psT", [128, 512])
    psTb = psT[:].bitcast(BF16)

    cst = p.sb("cst", [128, 5, 128], F32)
    E.dma(cst[:], consts_d)
    ident_f = cst[:, 0, :]; UTf = cst[:, 1, :]; SLf = cst[:, 2, :]; mstrict = cst[:, 3, :]; mincl = cst[:, 4, :]
    ident_b = p.sb("ident_b", [128, 128], BF16)
    E.copy(ident_b[:], ident_f, eng='dve')
    ones_f = p.sb("ones_f", [128, 128], F32)
    E.memset(ones_f[:], 1.0)
    ones_b = p.sb("ones_b", [128, 128], BF16)
    E.memset(ones_b[:], 1.0)
    blk_b = p.sb("blk_b", [128, 128], BF16)
    E.memset(blk_b[:], 0.0)
    E.memset(blk_b[0:64, 0:64], 1.0)
    E.memset(blk_b[64:128, 64:128], 1.0)
    rows = p.sb("rows", [128, 160], F32)
    E.dma(rows[:], rows_d)
    nw = p.sb("nw", [128, 5, 8], F32)
    E.dma(nw[:], nw_d)
    cmask = p.sb("cmask", [128, 12], F32)
    E.dma(cmask[:], cmask_d)
    modT = p.sb("modT", [128, 2, 48], F32)
    adab = p.sb("adab", [128, 2, 48], F32)
    E.dma(adab[:], ada_b2)
    cond = p.sb("cond", [128, 8], F32)
    E.dma(cond[:], condT_d)
    E.act(cond[:], cond[:], AF.Silu)
    negA = p.sb("negA", [128, 8], F32)
    E.act(negA[:], rows[:, 0:8], AF.Exp)
    E.ts(negA[:], negA[:], -1.0, MUL)
    dtb = rows[:, 8:16]; onw = rows[:, 16:80]; sinks = rows[:, 80:88]

    with nc.sbuf_tensor("s_adaw0", [128, 8, 1024], F32) as aw0, nc.sbuf_tensor("s_adaw1", [128, 8, 1024], F32) as aw1:
        aws = [aw0, aw1]
        i = 0
        for l in range(2):
            for s in range(6):
                aw = aws[i % 2]; i += 1
                E.dma(aw[:], ada_w[l, :, s * 1024:(s + 1) * 1024].rearrange("(k p) n -> p k n", p=128))
                for j in range(8):
                    col = s * 8 + j
                    for k in range(8):
                        E.mm(psD[:, col:col + 1], aw[:, k, j * 128:(j + 1) * 128], cond[:, k:k + 1],
                             start=(k == 0), stop=(k == 7))
            E.tt(modT[:, l, :], psD[:, 0:48], adab[:, l, :], ADD)
        p.barrier()
    AB = p.sb("AB", [128, 2, 6, 8], F32)
    for l in range(2):
        for (dst, sc_i, nwi) in ((0, 1, 2 * l), (3, 4, 2 * l + 1)):
            E.ts(AB[:, l, dst, :], modT[:, l, sc_i * 8:(sc_i + 1) * 8], 1.0, ADD)
            E.tt(AB[:, l, dst, :], AB[:, l, dst, :], nw[:, nwi, :], MUL)
        E.copy(AB[:, l, 1, :], modT[:, l, 0:8], eng='dve')
        E.copy(AB[:, l, 2, :], modT[:, l, 16:24], eng='dve')
        E.copy(AB[:, l, 4, :], modT[:, l, 24:32], eng='dve')
        E.copy(AB[:, l, 5, :], modT[:, l, 40:48], eng='dve')

    ctx = dict(nc=nc, p=p, E=E, cfg=cfg, dbg_cat=dbg_cat, xsT=xsT, maskrow=maskrow, w_in=w_in, w_out=w_out, convw_d=convw_d,
               bias_d=bias_d, x1T=x1T, x2T=x2T, x3T=x3T, outT=outT, AB=AB, nw=nw, rows=rows, negA=negA,
               cmask=cmask, ident_f=ident_f, ident_b=ident_b, UTf=UTf, SLf=SLf, mstrict=mstrict, mincl=mincl,
               ones_f=ones_f, ones_b=ones_b, blk_b=blk_b, dtb=dtb, onw=onw, sinks=sinks,
               ps=dict(A=psA, B=psB, C=psC, D=psD, E=psE, F=psF, G=psG, T=psT, Tb=psTb),
               pool_w=pool_w, pscale_d=pscale_d, poolfix_d=poolfix_d, r1w=r1w, r2w=r2w,
               wgate=wgate, wup=wup, wdown=wdown, modT=modT)

    if stop_after != 'prologue':
        phase_mixer0(ctx)
        p.barrier()
    if stop_after not in ('prologue', 'mixer0'):
        phase_moe(ctx, 0, x1T, cfg.NBE, x2T, final=False)
        p.barrier()
    if stop_after not in ('prologue', 'mixer0', 'moe0'):
        phase_pool(ctx)
        p.barrier()
    if stop_after not in ('prologue', 'mixer0', 'moe0', 'pool'):
        phase_moe(ctx, 1, x3T, cfg.NBO, outT, final=True)
    if dbg:
        dm = nc.dram_tensor("dbg_mod", [128, 2, 48], F32, kind="ExternalOutput").ap()
        E.dma(dm, modT[:], final=True)
    p.emit()
    return nc, in_names


def bcl(ap, n):
    sh = list(ap.shape)
    return ap.unsqueeze(len(sh)).broadcast_to(sh + [n])


def bcm(ap, n):
    sh = list(ap.shape)
    return ap.unsqueeze(1).broadcast_to([sh[0], n] + sh[1:])


def v3(ap, h):
    return ap.rearrange("p (h j) -> p h j", h=h)


def rsqrt_act(E, out, in_, scale, eps):
    E.act(out, in_, AF.Ln, bias=eps, scale=scale)
    E.act(out, out, AF.Exp, scale=-0.5)


class StopPhase(Exception):
    pass


def cutpoint(n):
    import os
    c = os.environ.get("MK_CUT", "")
    if c and float("0." + str(n)) >= float("0." + c):
        raise StopPhase()


def phase_mixer0(ctx):
    try:
        _phase_mixer0(ctx)
    except StopPhase:
        pass


def _phase_mixer0(ctx):
    from contextlib import ExitStack
    nc, p, E, cfg = ctx['nc'], ctx['p'], ctx['E'], ctx['cfg']
    ps = ctx['ps']
    psA, psB, psC, psD, psE, psF, psG, psT, psTb = (ps[k] for k in ('A', 'B', 'C', 'D', 'E', 'F', 'G', 'T', 'Tb'))
    AB, rows, negA, cmask = ctx['AB'], ctx['rows'], ctx['negA'], ctx['cmask']
    ident_f, ident_b, UTf, SLf, mstrict, mincl = (ctx[k] for k in ('ident_f', 'ident_b', 'UTf', 'SLf', 'mstrict', 'mincl'))
    ones_f, blk_b, dtb, onw, sinks = (ctx[k] for k in ('ones_f', 'blk_b', 'dtb', 'onw', 'sinks'))
    xsT, maskrow, x1T = ctx['xsT'], ctx['maskrow'], ctx['x1T']
    A1, B1, G1 = AB[:, 0, 0, :], AB[:, 0, 1, :], AB[:, 0, 2, :]
    NTILE = cfg.NBT // 4
    PRE = cfg.PRE
    first_q_tile = PRE // 4
    first_kb_tile = max(0, (PRE - 1) // 4)
    R = 2
    CH = F32 if os.environ.get('MK_CHAIN_F32', '1') == '1' else BF16
    F32R = mybir.dt.float32r
    USE_R = CH == F32 and os.environ.get('MK_F32R', '0') == '1'

    def rr(ap):
        return ap.bitcast(F32R) if USE_R else ap
    ident_c = ident_f if CH == F32 else ident_b[:]
    psTc = psT[:] if CH == F32 else psTb
    TW = 0 if CH == F32 else 1

    print('mixer0 sbuf remaining', nc.sbuf_bytes_remaining)
    with ExitStack() as es:
        def sb(name, shape, dt):
            return es.enter_context(nc.sbuf_tensor("s_" + name, list(shape), dt))

        win = sb("win_sb", [128, 8, P_IN], BF16)
        EXP = os.environ.get('MK_EXP', '0') == '1'
        wout = sb("wout_sb", [128, 8, D] if not EXP else [128, 8, 128], BF16)
        for k in range(8):
            E.dma(win[:, k, :], ctx['w_in'][k * 128:(k + 1) * 128, :], q='pool')
        for k in range(8):
            if not EXP:
                E.dma(wout[:, k, :], ctx['w_out'][k * 128:(k + 1) * 128, :], q='pool')
        convw = sb("convw", [128, 12, 4], F32)
        E.dma(convw[:], ctx['convw_d'])
        biast = sb("biast", [128, 8, 256], BF16)
        E.dma(biast[:], ctx['bias_d'], q='pool')
        xts = [sb("xt0", [128, 8, 512], F32)] * 2
        mk = sb("mk", [128, 512], F32)
        rstd = sb("rstd", [128, 512], F32)
        hT = sb("hT", [128, 8, 512], BF16)
        raws = [sb("raw0", [128, 515], F32)] * 2
        carry = sb("carry", [128, 12, 3], F32)
        E.memset(carry[:], 0.0)
        cvs = [sb("cv%d" % i, [128, 512], F32) for i in range(2)]
        sls = [sb("sl0", [128, 512], F32)] * 2
        sqs = cvs
        tmpx = sls
        sqb = sb("sqb", [128, 512], BF16)
        rn = rstd
        kTn = sb("kTn", [128, 4, 512], BF16)
        qTn = sb("qTn", [128, 4, 512], BF16)
        kTm = [sb("kTm%d" % i, [128, 4, 128], BF16) for i in range(2)]
        vT = sb("vT", [128, 4, 512], BF16)
        qbT = sb("qbT", [128, 4, 512], BF16)
        kbT = sb("kbT", [128, 4, 640], BF16)
        wkb = sb("wkb", [128, 8, 2, 128], BF16)
        for kv_ in range(2):
            for dup_ in range(2):
                E.copy(wkb[:, :, kv_, dup_ * 64:(dup_ + 1) * 64], win[:, :, 2576 + kv_ * 64:2576 + (kv_ + 1) * 64], eng='dve')
        pm = cmask[:, 2:4]
        pm8 = cmask[:, 4:12]
        E.memset(kbT[:], 0.0)
        smTs = [sb("smT%d" % i, [128, 4, 64], F32) for i in range(2)]
        stT = sb("stT", [128, 4, 24], F32)
        smt = sb("smt", [128, 64], F32)
        G3 = [sb("G3_0", [128, 4, 128], F32)] * 2
        Ef = [sb("Ef_0", [128, 4, 128], F32)] * 2
        Dsb = [sb("Dsb_0", [128, 4, 128], F32)] * 2
        Di = [sb("Di_0", [128, 4, 128], F32)] * 2
        Nb = [[sb("N%d_%d" % (a, i), [128, 4, 128], BF16) for i in range(2)] for a in range(2)]
        NTb = [[sb("NT%d_%d" % (a, i), [128, 4, 128], BF16) for i in range(2)] for a in range(2)]
        Yb = [[sb("Y%d_%d" % (a, i), [128, 4, 128], BF16) for i in range(2)] for a in range(2)]
        Ai = [sb("Ai_%d" % i, [128, 4, 128], F32) for i in range(2)]
        Z0f = [sb("Z0f_%d" % i, [128, 4, 128], F32) for i in range(2)]
        Z0T = [sb("Z0T_%d" % i, [128, 4, 128], F32) for i in range(2)]
        T1buf = [G3[0], Ef[0]]
        aqk = [sb("aqk_%d" % i, [128, 4, 128], BF16) for i in range(2)]
        bv = sb("bv", [128, 8, 64], CH)
        kbe = sb("kbe", [128, 8, 2, 64], CH)
        u_r = [sb("u_%d" % i, [128, 8, 64], F32) for i in range(R)]
        wT_r = [sb("wT_%d" % i, [128, 8, 128], BF16) for i in range(R)]
        kdd_r = [sb("kdd_%d" % i, [128, 8, 2, 64], BF16) for i in range(R)]
        aqkT_r = [sb("aqkT_%d" % i, [128, 8, 128], BF16) for i in range(R)]
        vb_r = [sb("vb_%d" % i, [128, 128], BF16) for i in range(R)]
        for i in range(R):
            E.memset(vb_r[i][:], 0.0)
        S = sb("S", [128, 8, 64], F32)
        S_tmp = sb("S_tmp", [128, 8, 64], F32)
        S_bf = sb("S_bf", [128, 8, 64], BF16)
        E.memset(S[:], 0.0)
        E.memset(S_bf[:], 0.0)
        vnew = sb("vnew", [128, 8, 64], BF16)
        o2s = sb("o2s", [128, 8, 64], F32)
        o_a = sb("o_a", [128, 8, 64], F32)
        osq = o2s
        z_s = o2s
        cat = sb("cat", [128, 16, 64], BF16)
        catT = sb("catT", [128, 8, 128], BF16)
        scs = [o2s[:].rearrange("p a b -> p (a b)").rearrange("p (a b) -> p a b", a=2)] * 2
        p_bf = sqb[:].rearrange("p (a b) -> p a b", a=2)
        pT = vnew[:].rearrange("p a (c d) -> p (a c) d", c=1).rearrange("p (a b) d -> p a (b d)", b=2)
        den = sb("den", [128, 16], F32)

        for ti in range(NTILE):
            xt = xts[ti % 2]
            c0 = ti * 512
            need_q = ti >= first_q_tile
            need_kb = ti >= first_kb_tile
            E.dma(xt[:], xsT[:, c0:c0 + 512].rearrange("(c p) t -> p c t", p=128))
            E.dma(mk[:], maskrow[0, c0:c0 + 512].partition_broadcast(128))
            for c in range(8):
                sq = sqs[c % 2]
                E.act(sq[:], xt[:, c, :], AF.Square)
                E.mm(psD[:], ones_f[:], sq[:], start=(c == 0), stop=(c == 7))
            rsqrt_act(E, rstd[:], psD[:], 1.0 / D, EPS)
            for c in range(8):
                tx = tmpx[c % 2]
                E.tt(tx[:], xt[:, c, :], rstd[:], MUL)
                E.act(tx[:], tx[:], AF.Identity, bias=B1[:, c:c + 1], scale=A1[:, c:c + 1])
                E.tt(hT[:, c, :], tx[:], mk[:], MUL, eng='pool')
            cutpoint(1)
            chunks = list(range(12)) if need_q else list(range(4, 12))
            for ci, cc in enumerate(chunks):
                raw = raws[ci % 2]
                for k in range(8):
                    E.mm(psD[:], win[:, k, cc * 128:(cc + 1) * 128], hT[:, k, :], start=(k == 0), stop=(k == 7))
                E.copy(raw[:, 0:3], carry[:, cc, :], eng='pool')
                E.copy(raw[:, 3:515], psD[:])
                E.copy(carry[:, cc, :], raw[:, 512:515], eng='pool')
                cv = cvs[ci % 2]
                E.ts(cv[:], raw[:, 0:512], convw[:, cc, 0:1], MUL)
                for j in range(1, 4):
                    E.stt(cv[:], raw[:, j:j + 512], convw[:, cc, j:j + 1], cv[:], MUL, ADD)
                if cc >= 8:
                    E.act(vT[:, cc - 8, :], cv[:], AF.Silu)
                else:
                    sl = sls[ci % 2]
                    E.act(sl[:], cv[:], AF.Silu)
                    E.act(sqb[:], sl[:], AF.Square)
                    E.mm(psE[:], blk_b[:], sqb[:])
                    rsqrt_act(E, rn[:], psE[:], 1.0, EPS)
                    if cc < 4:
                        E.stt(qTn[:, cc, :], sl[:], 0.125, rn[:], MUL, MUL)
                    else:
                        E.tt(kTn[:, cc - 4, :], sl[:], rn[:], MUL)
            if need_q:
                for c in range(4):
                    for k in range(8):
                        E.mm(psD[:], win[:, k, 2064 + c * 128:2064 + (c + 1) * 128], hT[:, k, :],
                             start=(k == 0), stop=(k == 7))
                    E.copy(qbT[:, c, :], psD[:])
            if need_kb:
                for kv in range(2):
                    for k in range(8):
                        E.mm(psD[:], wkb[:, k, kv, :], hT[:, k, :], start=(k == 0), stop=(k == 7))
                    for par in range(2):
                        E.ts(kbT[:, kv * 2 + par, 128:640], psD[:], pm[:, par:par + 1], MUL)

            cutpoint(2)
            smT = smTs[ti % 2]
            BA = psG[:, 0:128].rearrange("p (b c) -> p b c", b=4)
            for bi in range(4):
                for k in range(8):
                    E.mm(psG[:, bi * 32:bi * 32 + 16], hT[:, k, bi * 128:(bi + 1) * 128], win[:, k, 2048:2064],
                         start=(k == 0), stop=(k == 7))
            gT, betaT, egcT, ekdT, egtT, begeT, gcsT = (smT[:, :, i * 8:(i + 1) * 8] for i in range(7))
            E.act(stT[:, :, 0:8], BA[:, :, 0:8], AF.Exp, scale=-1.0)
            E.ts(stT[:, :, 0:8], stT[:, :, 0:8], 1.0, ADD)
            E.recip(betaT, stT[:, :, 0:8])
            E.tt(stT[:, :, 8:16], BA[:, :, 8:16], bcm(dtb, 4), ADD)
            E.act(stT[:, :, 8:16], stT[:, :, 8:16], AF.Exp)
            E.act(stT[:, :, 8:16], stT[:, :, 8:16], AF.Ln, bias=1.0)
            E.tt(gT, stT[:, :, 8:16], bcm(negA[:], 4), MUL)
            for bi in range(4):
                E.mm(psG[:, bi * 32 + 16:bi * 32 + 24], UTf, smT[:, bi, 0:8])
                E.mm(psG[:, bi * 32 + 24:bi * 32 + 32], ones_f[:], smT[:, bi, 0:8])
            E.copy(gcsT, BA[:, :, 16:24])
            E.act(egcT, BA[:, :, 16:24], AF.Exp)
            E.act(egtT, BA[:, :, 24:32], AF.Exp)
            E.tt(stT[:, :, 16:24], BA[:, :, 24:32], gcsT, SUB)
            E.act(ekdT, stT[:, :, 16:24], AF.Exp)
            E.tt(begeT, betaT, egcT, MUL)

            def blk_pre(bi):
                    b = ti * 4 + bi
                    t0 = bi * 128
                    own = b >= PRE
                    r = b % R
                    g, beta, egc, ekd, egt, bege, gcs = (smT[:, bi, i * 8:(i + 1) * 8] for i in range(7))
                    kdd = kdd_r[r]

                    for c in range(4):
                        E.tr(psTb[:, c * 128:(c + 1) * 128], vT[:, c, t0:t0 + 128], ident_b[:])
                    for c in range(4):
                        E.tr(psTb[:, 512 + c * 128:512 + (c + 1) * 128], kTn[:, c, t0:t0 + 128], ident_b[:])
                    vtok = v3(psTb[:, 0:512], 8)
                    ktok = v3(psTb[:, 512:1024], 8)
                    E.tt(bv[:], vtok, bcl(beta, 64), MUL)
                    E.tt(kbe[:, :, 0, :], ktok, bcl(bege, 64), MUL)
                    E.copy(kbe[:, :, 1, :], kbe[:, :, 0, :], eng='pool')
                    kdd = kdd_r[r]
                    E.tt(kdd[:, :, 0, :], ktok, bcl(ekd, 64), MUL)
                    E.copy(kdd[:, :, 1, :], kdd[:, :, 0, :], eng='pool')
                    yield
                    for par in range(2):
                        E.ts(kTm[par][:], kTn[:, :, t0:t0 + 128], pm[:, par:par + 1], MUL)
                    for hh in range(2):
                        h0 = hh * 4
                        E.tt(G3[hh][:], bcm(UTf, 4), bcl(g[:, h0:h0 + 4], 128), MUL, eng='pool')
                        for j in range(4):
                            E.mm(psA[:, j * 128:(j + 1) * 128], G3[hh][:, j, :], SLf)
                        E.act(Ef[hh][:], v3(psA[:], 4), AF.Exp)
                        E.tt(Dsb[hh][:], Ef[hh][:], bcm(mstrict, 4), MUL, eng='pool')
                        E.tt(Dsb[hh][:], Dsb[hh][:], bcl(beta[:, h0:h0 + 4], 128), MUL, eng='pool')
                        for j in range(4):
                            h = h0 + j
                            c, r0 = h // 2, (h % 2) * 64
                            E.mm(psB[:, j * 128:(j + 1) * 128], kTn[:, c, t0:t0 + 128], kTm[h % 2][:, c, :])
                        yield
                        N0f = Ai[hh]
                        E.tt(N0f[:], v3(psB[:], 4), Dsb[hh][:], MUL)
                        N0 = Nb[0][hh]
                        E.copy(N0[:], N0f[:])
                        E.tt(N0f[:], N0f[:], bcm(ident_f, 4), ADD)
                        if own:
                            E.tt(Di[hh][:], Ef[hh][:], bcm(mincl, 4), MUL, eng='pool')
                            for j in range(4):
                                h = h0 + j
                                c, r0 = h // 2, (h % 2) * 64
                                E.mm(psC[:, j * 128:(j + 1) * 128], qTn[:, c, t0:t0 + 128], kTm[h % 2][:, c, :])
                            E.tt(aqk[hh][:], v3(psC[:], 4), Di[hh][:], MUL)
                            for j in range(4):
                                E.tr(psTb[:, hh * 512 + j * 128:hh * 512 + (j + 1) * 128], aqk[hh][:, j, :], ident_b[:])
                            E.copy(aqkT_r[r][:, h0:h0 + 4, :], v3(psTb[:, hh * 512:(hh + 1) * 512], 4))
                        for j in range(4):
                            E.tr(psTb[:, hh * 512 + j * 128:hh * 512 + (j + 1) * 128], N0[:, j, :], ident_b[:])
                        ntp = v3(psTb[:, hh * 512:(hh + 1) * 512], 4)
                        NT0 = NTb[0][hh]
                        E.copy(NT0[:], ntp)
                        Y0 = Yb[0][hh]
                        E.ts(Y0[:], NT0[:], -1.0, MUL)
                        E.tt(Y0[:], Y0[:], bcm(ident_f, 4), ADD)
                        yield

            def blk_chain(bi):
                    b = ti * 4 + bi
                    t0 = bi * 128
                    own = b >= PRE
                    r = b % R
                    g, beta, egc, ekd, egt, bege, gcs = (smT[:, bi, i * 8:(i + 1) * 8] for i in range(7))
                    kdd = kdd_r[r]

                    PA, PB, PC = (psA, psE), (psB, psF), (psC, psG)
                    cur = 0
                    for lvl in range(1, 7):
                        for hh in range(2):
                            Nc, NTc = Nb[cur][hh], NTb[cur][hh]
                            for j in range(4):
                                E.mm(PA[hh][:, j * 128:(j + 1) * 128], NTc[:, j, :], Nc[:, j, :])
                            if lvl < 6:
                                for j in range(4):
                                    E.mm(PB[hh][:, j * 128:(j + 1) * 128], Nc[:, j, :], NTc[:, j, :])
                        for hh in range(2):
                            E.copy(Nb[1 - cur][hh][:], v3(PA[hh][:], 4))
                            if lvl < 6:
                                E.copy(NTb[1 - cur][hh][:], v3(PB[hh][:], 4), eng='dve')
                        for hh in range(2):
                            Nn, Yc = Nb[1 - cur][hh], Yb[cur][hh]
                            for j in range(4):
                                E.mm(PC[hh][:, j * 128:(j + 1) * 128], Nn[:, j, :], Yc[:, j, :])
                        for hh in range(2):
                            E.tt(Yb[1 - cur][hh][:], v3(PC[hh][:], 4), Yb[cur][hh][:], ADD)
                        cur = 1 - cur
                    NEWTON = int(os.environ.get('MK_NEWTON', '1'))
                    for hh in range(2):
                        E.copy(Z0f[hh][:], Yb[cur][hh][:])
                    for it in range(NEWTON):
                        for hh in range(2):
                            for j in range(4):
                                E.mm(PA[hh][:, j * 128:(j + 1) * 128], Ai[hh][:, j, :], Z0f[hh][:, j, :])
                            for j in range(4):
                                E.tr(PB[hh][:, j * 128:(j + 1) * 128], Z0f[hh][:, j, :], ident_f)
                        for hh in range(2):
                            E.copy(Z0T[hh][:], v3(PB[hh][:], 4), eng='dve')
                        T1s = Yb16f = None
                        for hh in range(2):
                            E.copy(T1buf[hh][:], v3(PA[hh][:], 4))
                        for hh in range(2):
                            for j in range(4):
                                E.mm(PC[hh][:, j * 128:(j + 1) * 128], Z0T[hh][:, j, :], T1buf[hh][:, j, :])
                        for hh in range(2):
                            E.stt(Z0f[hh][:], Z0f[hh][:], 2.0, v3(PC[hh][:], 4), MUL, SUB)
                    for hh in range(2):
                        h0 = hh * 4
                        Yf = Z0f[hh]
                        for j in range(4):
                            h = h0 + j
                            E.mm(psD[:, h * 64:(h + 1) * 64], rr(Yf[:, j, :]), rr(bv[:, h, :]))
                        for j in range(4):
                            h = h0 + j
                            E.mm(PC[hh][:, j * 128:(j + 1) * 128], rr(kbe[:, h, :, :].rearrange("p a b -> p (a b)")), rr(Yf[:, j, :]))
                        E.copy(wT_r[r][:, h0:h0 + 4, :], v3(PC[hh][:], 4))
                    E.copy(u_r[r][:], v3(psD[:], 8))

            def blk_rec(bi):
                    b = ti * 4 + bi
                    t0 = bi * 128
                    own = b >= PRE
                    r = b % R
                    g, beta, egc, ekd, egt, bege, gcs = (smT[:, bi, i * 8:(i + 1) * 8] for i in range(7))
                    kdd = kdd_r[r]

                    for h in range(8):
                        E.mm(psE[:, h * 64:(h + 1) * 64], wT_r[r][:, h, :], S_bf[:, h, :])
                    E.tt(vnew[:], u_r[r][:], v3(psE[:], 8), SUB)
                    yield
                    if own:
                        for h in range(8):
                            c, r0 = h // 2, (h % 2) * 64
                            E.mm(psF[:, h * 64:(h + 1) * 64], qTn[:, c, t0:t0 + 128], S_bf[:, h, :])
                        for h in range(8):
                            E.mm(psG[:, h * 64:(h + 1) * 64], aqkT_r[r][:, h, :], vnew[:, h, :])
                    E.tt(S_tmp[:], S[:], bcl(egt, 64), MUL, eng='pool')
                    for h in range(8):
                        E.mm(psE[:, h * 64:(h + 1) * 64], kdd[:, h, :, :].rearrange("p a b -> p (a b)"), vnew[:, h, :])
                    yield
                    if own:
                        E.copy(o2s[:], v3(psG[:], 8))
                        E.tt(o_a[:], v3(psF[:], 8), bcl(egc, 64), MUL)
                        E.tt(o_a[:], o_a[:], o2s[:], ADD, eng='pool')
                    E.tt(S[:], S_tmp[:], v3(psE[:], 8), ADD)
                    E.tt(S_bf[:], S[:], bcl(pm8, 64), MUL)
                    yield
                    if b >= PRE - 1:
                        for k in range(8):
                            E.mm(psG[:, 0:128], hT[:, k, t0:t0 + 128], win[:, k, 2704:2832], start=(k == 0), stop=(k == 7))
                        E.copy(vb_r[r][:], psG[:, 0:128])
                    if not own:
                        return
                    E.tt(osq[:], o_a[:], o_a[:], MUL, eng='pool')
                    E.red(den[:, 8:16], osq[:], ADD)
                    rsqrt_act(E, den[:, 8:16], den[:, 8:16], 1.0 / 64, EPS)
                    E.tt(o_a[:], o_a[:], bcl(den[:, 8:16], 64), MUL)
                    E.tt(o_a[:], o_a[:], bcm(onw, 8), MUL, eng='pool')
                    for k in range(8):
                        E.mm(psF[:], hT[:, k, t0:t0 + 128], win[:, k, 1536:2048], start=(k == 0), stop=(k == 7))
                    E.act(z_s[:], v3(psF[:], 8), AF.Silu)
                    E.tt(cat[:, 0:8, :], o_a[:], z_s[:], MUL)
                    yield
                    vprev, vcur = vb_r[(b - 1) % R], vb_r[r]
                    for gi in range(4):
                        sc = scs[gi % 2]
                        for hh in range(2):
                            h = gi * 2 + hh
                            kv = h // 4
                            E.mm(psF[:, hh * 256:(hh + 1) * 256], qbT[:, h // 2, t0:t0 + 128], kbT[:, kv * 2 + h % 2, t0:t0 + 256])
                        E.stt(sc[:], v3(psF[:], 2), 0.125, biast[:, gi * 2:gi * 2 + 2, :], MUL, ADD)
                        if b == PRE + 1:
                            E.ts(sc[:, :, 0:128], sc[:, :, 0:128], cmask[:, 0:1], ADD)
                        mx = smt[:, 32 + gi * 2:32 + gi * 2 + 2]
                        E.red(mx, sc[:], MAX)
                        E.tt(mx, mx, sinks[:, gi * 2:gi * 2 + 2], MAX)
                        nmx = smt[:, 40 + gi * 2:40 + gi * 2 + 2]
                        E.ts(nmx, mx, -1.0, MUL)
                        for hh in range(2):
                            h = gi * 2 + hh
                            E.act(p_bf[:, hh, :], sc[:, hh, :], AF.Exp, bias=nmx[:, hh:hh + 1], accum=den[:, h:h + 1])
                        for hh in range(2):
                            for kb in range(2):
                                E.tr(psTb[:, (hh * 2 + kb) * 128:(hh * 2 + kb + 1) * 128], p_bf[:, hh, kb * 128:(kb + 1) * 128],
                                     ident_b[:])
                        E.copy(pT[:], v3(psTb[:, 0:512], 4))
                        yield
                        for hh in range(2):
                            h = gi * 2 + hh
                            kv = h // 4
                            E.mm(psE[:, h * 64:(h + 1) * 64], pT[:, hh * 2, :], vprev[:, kv * 64:(kv + 1) * 64],
                                 start=True, stop=False)
                            E.mm(psE[:, h * 64:(h + 1) * 64], pT[:, hh * 2 + 1, :], vcur[:, kv * 64:(kv + 1) * 64],
                                 start=False, stop=True)
                    E.tt(smt[:, 48:56], sinks, smt[:, 32:40], SUB)
                    E.act(smt[:, 48:56], smt[:, 48:56], AF.Exp)
                    E.tt(den[:, 0:8], den[:, 0:8], smt[:, 48:56], ADD)
                    E.recip(den[:, 0:8], den[:, 0:8])
                    E.tt(cat[:, 8:16, :], v3(psE[:], 8), bcl(den[:, 0:8], 64), MUL)
                    if ctx['dbg_cat'] is not None:
                        E.dma(ctx['dbg_cat'][(b - PRE) * 128:(b - PRE + 1) * 128, :], cat[:].rearrange("p a b -> p (a b)"), q='pool')
                    catf = cat[:].rearrange("p a b -> p (a b)")
                    for k in range(8):
                        E.tr(psTb[:, k * 128:(k + 1) * 128], catf[:, k * 128:(k + 1) * 128], ident_b[:])
                    E.copy(catT[:], v3(psTb[:, 0:1024], 8))
                    if not EXP:
                        for m in range(8):
                            for k in range(8):
                                E.mm(psD[:, 0:128], wout[:, k, m * 128:(m + 1) * 128], catT[:, k, :],
                                     start=(k == 0), stop=(k == 7))
                            E.stt(xt[:, m, t0:t0 + 128], psD[:, 0:128], G1[:, m:m + 1], xt[:, m, t0:t0 + 128], MUL, ADD)
                    yield

            def run_all(g):
                for _ in g:
                    pass

            def interleave(g1, g2):
                d1 = d2 = False
                while not (d1 and d2):
                    if not d1:
                        try:
                            next(g1)
                        except StopIteration:
                            d1 = True
                    if not d2:
                        try:
                            next(g2)
                        except StopIteration:
                            d2 = True

            PIPE = os.environ.get('MK_PIPE', '1') == '1'
            run_all(blk_pre(0))
            for bi in range(4):
                blk_chain(bi)
                if bi < 3 and PIPE:
                    interleave(blk_rec(bi), blk_pre(bi + 1))
                else:
                    run_all(blk_rec(bi))
                    if bi < 3:
                        run_all(blk_pre(bi + 1))

            if need_kb:
                E.copy(kbT[:, :, 0:128], kbT[:, :, 512:640], eng='pool')
            lo_b = max(PRE, ti * 4)
            if lo_b < ti * 4 + 4 and not EXP:
                lo = (lo_b - ti * 4) * 128
                n = 512 - lo
                e0 = (lo_b - PRE) * 128
                E.dma(x1T[:, e0:e0 + n].rearrange("(c p) t -> p c t", p=128), xt[:, :, lo:512])


def _t5_bucket_table():
    import math
    qi = np.arange(128)[:, None]
    ki = np.arange(256)[None, :]
    dist = qi + 128 - ki
    n = np.maximum(dist, 0)
    nf = np.maximum(n, 1).astype(np.float32)
    large = 16 + (np.log(nf / 16) / np.float32(math.log(128 / 16)) * 16).astype(np.int32)
    large = np.minimum(large, 31)
    bucket = np.where(n < 16, n, large)
    valid = (dist >= 0) & (dist < 128)
    return bucket, valid


def _static_consts():
    i = np.arange(128)
    same = np.ones((128, 128), bool)
    c = np.zeros((128, 5, 128), np.float32)
    c[:, 0, :] = np.eye(128)
    c[:, 1, :] = (i[:, None] <= i[None, :])
    c[:, 2, :] = (i[:, None] > i[None, :])
    c[:, 3, :] = (i[:, None] > i[None, :])
    c[:, 4, :] = (i[:, None] >= i[None, :])
    return c


def fm(v):
    v = np.asarray(v, np.float32)
    return np.ascontiguousarray(v.reshape(-1, 128).T)


def prep_inputs(inp, cfg):
    f32 = np.float32
    x = np.asarray(inp['x'], f32)
    B, L, _ = x.shape
    NT, NOWN = cfg.NT, cfg.NOWN
    bucket, valid = _t5_bucket_table()
    rel_bias = np.asarray(inp['rel_bias'], f32)
    bt = rel_bias[bucket]
    bt = np.where(valid[:, :, None], bt, f32(NEG))
    bias_tab = np.ascontiguousarray(bt.transpose(0, 2, 1))
    consts = _static_consts()
    rows = np.concatenate([inp['ab_a_log'][0], inp['ab_dt_bias'][0], inp['ab_onorm_w'][0], inp['ab_sinks'][0],
                           inp['r1_b'][0], inp['r2_b'][0], inp['r1_b'][1], inp['r2_b'][1]]).astype(f32)
    rows = np.ascontiguousarray(np.broadcast_to(rows[None, :], (128, rows.shape[0])))
    nw = np.stack([fm(inp['norm_mix_w'][0]), fm(inp['norm_ffn_w'][0]), fm(inp['norm_mix_w'][1]),
                   fm(inp['norm_ffn_w'][1]), fm(inp['final_norm_w'])], axis=1)
    ada_b2 = np.stack([fm(inp['ada_b'][0]), fm(inp['ada_b'][1])], axis=1)
    convw = np.ascontiguousarray(np.asarray(inp['ab_conv_w'][0], f32).T.reshape(12, 128, 4).transpose(1, 0, 2))
    pscale = fm(inp['pool_scale'][0])
    shared = dict(
        ada_w=np.asarray(inp['ada_w'], f32), ada_b2=ada_b2, nw=nw, w_in=np.asarray(inp['ab_w_in'][0], f32),
        w_out=np.asarray(inp['ab_w_out'][0], f32), convw=convw, rows=rows, bias_tab=bias_tab,
        pool_w=np.asarray(inp['pool_w'][0], f32), pscale=pscale, r1w=np.asarray(inp['r1_w'], f32),
        r2w=np.asarray(inp['r2_w'], f32), moe_w_gate=np.asarray(inp['moe_w_gate'], f32),
        moe_w_up=np.asarray(inp['moe_w_up'], f32), moe_w_down=np.asarray(inp['moe_w_down'], f32), consts=consts)
    maps = []
    wins = (2, 2, 4, 4, 8, 8, 16, 16)
    for core in range(8):
        b, s = core // 4, core % 4
        nreal = (s + 1) * NOWN
        xsT = np.zeros((D, NT), f32)
        xsT[:, NT - nreal:] = x[b, :nreal, :].T
        maskrow = np.zeros((1, NT), f32)
        maskrow[0, NT - nreal:] = 1.0
        cmask = np.zeros((128, 12), f32)
        cmask[:64, 2] = 1.0
        cmask[64:, 3] = 1.0
        for hh_ in range(8):
            cmask[:, 4 + hh_] = cmask[:, 2 + hh_ % 2]
        cmask[:, 0] = NEG if s == 0 else 0.0
        cmask[:, 1] = 0.0 if s == 0 else 1.0
        poolfix = np.zeros((128, 8, 16), f32)
        for c in range(8):
            w = wins[c]
            t = np.arange(16)
            cnt = np.minimum(t + 1, w) if s == 0 else np.full(16, w)
            poolfix[:, c, :] = (1.0 / cnt.astype(f32))[None, :]
        m = dict(shared)
        m.update(xsT=xsT, maskrow=maskrow, condT=fm(inp['c'][b]), cmask=cmask, poolfix=poolfix)
        maps.append(m)
    return maps


def phase_moe(ctx, layer, src, nblk, dst, final):
    from contextlib import ExitStack
    nc, p, E, cfg = ctx['nc'], ctx['p'], ctx['E'], ctx['cfg']
    ps = ctx['ps']
    psA, psB, psC, psD, psE, psF, psG, psT = (ps[k] for k in ('A', 'B', 'C', 'D', 'E', 'F', 'G', 'T'))
    AB, rows, nw = ctx['AB'], ctx['rows'], ctx['nw']
    ident_f, ones_f = ctx['ident_f'], ctx['ones_f']
    A2, B2, G2 = AB[:, layer, 3, :], AB[:, layer, 4, :], AB[:, layer, 5, :]
    r1b = rows[:, 88:92] if layer == 0 else rows[:, 124:128]
    r2b = rows[:, 92:124] if layer == 0 else rows[:, 128:160]
    wg_d, wu_d, wd_d = ctx['wgate'], ctx['wup'], ctx['wdown']
    npass = (nblk + 16) // 17
    per = (nblk + npass - 1) // npass
    passes = [list(range(i, min(i + per, nblk))) for i in range(0, nblk, per)]
    PB = max(len(x) for x in passes)

    with ExitStack() as es:
        def sb(name, shape, dt):
            return es.enter_context(nc.sbuf_tensor("s_m%d_" % layer + name, list(shape), dt))

        rw = sb("rw", [128, 8, 36], F32)
        E.dma(rw[:, :, 0:4], ctx['r1w'][layer].rearrange("(k p) n -> p k n", p=128))
        E.dma(rw[:, :, 4:36], ctx['r2w'][layer].rearrange("(k p) n -> p k n", p=128))
        h2 = sb("h2", [128, 8, PB * 128], BF16)
        acc = sb("acc", [128, PB, D], F32)
        Gt = sb("Gt", [128, PB, 32], F32)
        xt = sb("xt", [128, 8, 512], F32)
        stg = [sb("stg%d" % i, [128, 2, 512], F32) for i in range(4)]

        def hfc(c):
            return stg[c // 2][:, c % 2, :]
        sq = sb("sq", [128, 512], F32)
        rstd = sb("rstd", [128, 512], F32)
        wgs = [sb("wg%d" % i, [128, 8, 512], BF16) for i in range(2)]
        wus = [sb("wu%d" % i, [128, 8, 512], BF16) for i in range(2)]
        wds = [sb("wd%d" % i, [128, 4, D], BF16) for i in range(2)]
        sg = [sb("sg%d" % i, [128, 512], F32) for i in range(2)]
        he = sb("he", [128, 4, 512], BF16)
        rtT = sb("rtT", [128, 4, 128], F32)
        qT = sb("qT", [128, 4, 64], F32)

        class Loader:
            def __init__(self):
                self.n = 0

            def items(self, e, slot):
                it = []
                for kp in range(4):
                    it.append((wgs[slot][:, 2 * kp:2 * kp + 2, :],
                               wg_d[layer, e][kp * 256:(kp + 1) * 256, :].rearrange("(k p) n -> p k n", p=128)))
                for kp in range(4):
                    it.append((wus[slot][:, 2 * kp:2 * kp + 2, :],
                               wu_d[layer, e][kp * 256:(kp + 1) * 256, :].rearrange("(k p) n -> p k n", p=128)))
                for k in range(4):
                    it.append((wds[slot][:, k, :].rearrange("p (a b) -> p a b", a=2),
                               wd_d[layer, e][k * 128:(k + 1) * 128, :].rearrange("p (a b) -> p a b", a=2)))
                return it

            def start(self, e, slot):
                self.it = self.items(e, slot)
                self.di = 0
                self.ci = 0
                self.base = self.n
                for _ in range(3):
                    self.dma_next()

            def dma_next(self):
                if self.di < len(self.it):
                    st = stg[(self.base + self.di) % 4]
                    E.dma(st[:], self.it[self.di][1])
                    self.di += 1

            def step(self):
                if self.ci < len(self.it):
                    self.dma_next()
                    st = stg[(self.base + self.ci) % 4]
                    E.copy(self.it[self.ci][0], st[:])
                    self.ci += 1
                    self.n += 1
                    return True
                return False

            def flush(self):
                while self.step():
                    pass

        ld = Loader()

        for blocks in passes:
            nb = len(blocks)
            tiles = [blocks[i:i + 4] for i in range(0, nb, 4)]
            for tl in tiles:
                n = len(tl) * 128
                c0 = tl[0] * 128
                l0 = (tl[0] - blocks[0]) * 128
                E.dma(xt[:, :, 0:n], src[:, c0:c0 + n].rearrange("(c p) t -> p c t", p=128))
                for c in range(8):
                    E.act(sq[:, 0:n], xt[:, c, 0:n], AF.Square)
                    E.mm(psD[:, 0:n], ones_f[:], sq[:, 0:n], start=(c == 0), stop=(c == 7))
                rsqrt_act(E, rstd[:, 0:n], psD[:, 0:n], 1.0 / D, EPS)
                for c in range(8):
                    E.tt(hfc(c)[:, 0:n], xt[:, c, 0:n], rstd[:, 0:n], MUL)
                    E.act(hfc(c)[:, 0:n], hfc(c)[:, 0:n], AF.Identity, bias=B2[:, c:c + 1], scale=A2[:, c:c + 1])
                    E.copy(h2[:, c, l0:l0 + n], hfc(c)[:, 0:n], eng='dve')
                nbt = len(tl)
                lb0 = tl[0] - blocks[0]
                for bi in range(nbt):
                    for k in range(8):
                        E.mm(psE[:, bi * 36:(bi + 1) * 36], hfc(k)[:, bi * 128:(bi + 1) * 128], rw[:, k, :],
                             start=(k == 0), stop=(k == 7))
                L = psE[:, 0:nbt * 36].rearrange("p (b c) -> p b c", b=nbt)
                Rr = rtT[:, 0:nbt, :]
                Qq = qT[:, 0:nbt, :]
                l1 = Rr[:, :, 0:4]; l2 = Rr[:, :, 4:36]
                E.tt(l1, L[:, :, 0:4], bcm(r1b, nbt), ADD)
                E.tt(l2, L[:, :, 4:36], bcm(r2b, nbt), ADD)
                m1 = Rr[:, :, 36]
                E.red(m1, l1, MAX)
                oh = Rr[:, :, 40:44]
                E.tt(oh, l1, bcl(m1, 4), ALU.is_equal)
                ex = Rr[:, :, 44:48]
                E.tt(ex, l1, bcl(m1, 4), SUB)
                E.act(ex, ex, AF.Exp)
                ptop = Rr[:, :, 39]
                E.red(ptop, ex, ADD)
                E.recip(ptop, ptop)
                pen = Rr[:, :, 48:52]
                E.ts(pen, oh, 1.0, SUB, 1.0e30, MUL)
                lm = Rr[:, :, 52:84]
                E.tt(lm.rearrange("p b (g e) -> p b g e", g=4), l2.rearrange("p b (g e) -> p b g e", g=4),
                     pen.unsqueeze(3).broadcast_to([128, nbt, 4, 8]), ADD)
                t1 = Rr[:, :, 84]
                E.red(t1, lm, MAX)
                o1 = Rr[:, :, 88:120]
                E.tt(o1, lm, bcl(t1, 32), ALU.is_equal)
                lm2 = Qq[:, :, 0:32]
                E.stt(lm2, o1, -1.0e30, lm, MUL, ADD)
                t2 = Rr[:, :, 85]
                E.red(t2, lm2, MAX)
                o2 = Qq[:, :, 32:64]
                E.tt(o2, lm2, bcl(t2, 32), ALU.is_equal)
                dd = Rr[:, :, 86]
                E.tt(dd, t2, t1, SUB)
                E.act(dd, dd, AF.Exp)
                w1 = Rr[:, :, 87]
                E.ts(w1, dd, 1.0, ADD)
                E.recip(w1, w1)
                w2 = Rr[:, :, 120]
                E.tt(w2, dd, w1, MUL)
                E.tt(w1, w1, ptop, MUL)
                E.tt(w2, w2, ptop, MUL)
                Gv = Gt[:, lb0:lb0 + nbt, :]
                E.tt(lm2, o2, bcl(w2, 32), MUL)
                E.tt(Gv, o1, bcl(w1, 32), MUL)
                E.tt(Gv, Gv, lm2, ADD)
            for e in range(32):
                slot = e % 2
                if e == 0:
                    ld.start(0, 0)
                    ld.flush()
                if e + 1 < 32:
                    ld.start(e + 1, 1 - slot)
                wg, wu, wd = wgs[slot], wus[slot], wds[slot]
                for tl in tiles:
                    n = len(tl) * 128
                    l0 = (tl[0] - blocks[0]) * 128
                    for m in range(4):
                        pg, pu = (psA, psB) if m % 2 == 0 else (psC, psD)
                        for k in range(8):
                            E.mm(pg[:, 0:n], wg[:, k, m * 128:(m + 1) * 128], h2[:, k, l0:l0 + n], start=(k == 0), stop=(k == 7))
                        for k in range(8):
                            E.mm(pu[:, 0:n], wu[:, k, m * 128:(m + 1) * 128], h2[:, k, l0:l0 + n], start=(k == 0), stop=(k == 7))
                        s_ = sg[m % 2]
                        E.act(s_[:, 0:n], pg[:, 0:n], AF.Silu)
                        E.tt(he[:, m, 0:n], s_[:, 0:n], pu[:, 0:n], MUL)
                        if e + 1 < 32:
                            ld.step()
                    for bi, b in enumerate(tl):
                        lb = b - blocks[0]
                        for half in range(2):
                            po = (psE, psF, psG, psT)[(bi * 2 + half) % 4]
                            for k in range(4):
                                E.mm(po[:], he[:, k, bi * 128:(bi + 1) * 128], wd[:, k, half * 512:(half + 1) * 512],
                                     start=(k == 0), stop=(k == 3))
                            a_ = acc[:, lb, half * 512:(half + 1) * 512]
                            if e == 0:
                                E.ts(a_, po[:], Gt[:, lb, e:e + 1], MUL)
                            else:
                                E.stt(a_, po[:], Gt[:, lb, e:e + 1], a_, MUL, ADD)
                if e + 1 < 32:
                    ld.flush()
            for tl in tiles:
                n = len(tl) * 128
                c0 = tl[0] * 128
                E.dma(xt[:, :, 0:n], src[:, c0:c0 + n].rearrange("(c p) t -> p c t", p=128))
                for m in range(8):
                    pt = (psA, psB)[m % 2]
                    for bi, b in enumerate(tl):
                        lb = b - blocks[0]
                        E.tr(pt[:, bi * 128:(bi + 1) * 128], acc[:, lb, m * 128:(m + 1) * 128], ident_f)
                    E.stt(xt[:, m, 0:n], pt[:, 0:n], G2[:, m:m + 1], xt[:, m, 0:n], MUL, ADD)
                if final:
                    for c in range(8):
                        E.act(sq[:, 0:n], xt[:, c, 0:n], AF.Square)
                        E.mm(psD[:, 0:n], ones_f[:], sq[:, 0:n], start=(c == 0), stop=(c == 7))
                    rsqrt_act(E, rstd[:, 0:n], psD[:, 0:n], 1.0 / D, EPS)
                    for c in range(8):
                        E.stt(xt[:, c, 0:n], xt[:, c, 0:n], nw[:, 4, c:c + 1], rstd[:, 0:n], MUL, MUL)
                E.dma(dst[:, c0:c0 + n].rearrange("(c p) t -> p c t", p=128), xt[:, :, 0:n], final=final)


def phase_pool(ctx):
    from contextlib import ExitStack
    nc, p, E, cfg = ctx['nc'], ctx['p'], ctx['E'], ctx['cfg']
    ps = ctx['ps']
    psA, psD = ps['A'], ps['D']
    AB, cmask, ones_f = ctx['AB'], ctx['cmask'], ctx['ones_f']
    A1, B1, G1 = AB[:, 1, 0, :], AB[:, 1, 1, :], AB[:, 1, 2, :]
    x2T, x3T = ctx['x2T'], ctx['x3T']
    wins = (2, 4, 8, 16)
    with ExitStack() as es:
        def sb(name, shape, dt):
            return es.enter_context(nc.sbuf_tensor("s_pl_" + name, list(shape), dt))

        pw = sb("pw", [128, 4, 2, 256], BF16)
        for g in range(4):
            E.dma(pw[:, g, :, :], ctx['pool_w'][g].rearrange("(k p) n -> p k n", p=128), q='pool')
        psc = sb("psc", [128, 8], F32)
        E.dma(psc[:], ctx['pscale_d'])
        gps = sb("gps", [128, 8], F32)
        E.tt(gps[:], psc[:], G1, MUL)
        pfix = sb("pfix", [128, 8, 16], F32)
        E.dma(pfix[:], ctx['poolfix_d'])
        W = 528
        xt = sb("xt", [128, 8, W], F32)
        h = sb("h", [128, 8, W], F32)
        sa = sb("sa", [128, 2, W], F32)
        sbb = sb("sbb", [128, 2, W], F32)
        sq = sb("sq", [128, W], F32)
        rstd = sb("rstd", [128, W], F32)
        pl = sb("pl", [128, 8, 512], BF16)
        tf = sb("tf", [128, 2, 16], F32)
        ntile = cfg.NOWN // 512 if cfg.NOWN >= 512 else 1
        TW = min(512, cfg.NOWN)
        for ti in range(ntile):
            c0 = 128 + ti * TW - 16
            n = TW + 16
            E.dma(xt[:, :, 0:n], x2T[:, c0:c0 + n].rearrange("(c p) t -> p c t", p=128))
            for (a, bnd) in ((0, 16), (16, n)):
                for c in range(8):
                    E.act(sq[:, a:bnd], xt[:, c, a:bnd], AF.Square)
                    E.mm(psD[:, 0:bnd - a], ones_f[:], sq[:, a:bnd], start=(c == 0), stop=(c == 7))
                rsqrt_act(E, rstd[:, a:bnd], psD[:, 0:bnd - a], 1.0 / D, EPS)
            for c in range(8):
                E.tt(h[:, c, 0:n], xt[:, c, 0:n], rstd[:, 0:n], MUL)
                E.act(h[:, c, 0:n], h[:, c, 0:n], AF.Identity, bias=B1[:, c:c + 1], scale=A1[:, c:c + 1])
            if ti == 0:
                E.ts(h[:, :, 0:16], h[:, :, 0:16], cmask[:, 1:2], MUL)
            for g in range(4):
                cc = slice(2 * g, 2 * g + 2)
                cur = h[:, cc, :]
                bufs = [sa, sbb]
                step, bi_, vs = 1, 0, 0
                while step < wins[g]:
                    nxt = bufs[bi_ % 2]; bi_ += 1
                    E.tt(nxt[:, :, vs + step:n], cur[:, :, vs + step:n], cur[:, :, vs:n - step], ADD)
                    cur = nxt[:, :, :]
                    vs += step
                    step *= 2
                E.stt(pl[:, cc, 0:TW], cur[:, :, 16:n], 1.0 / wins[g], h[:, cc, 16:n], MUL, SUB)
                if ti == 0:
                    E.tt(tf[:], cur[:, :, 16:32], pfix[:, cc, :], MUL)
                    E.tt(pl[:, cc, 0:16], tf[:], h[:, cc, 16:32], SUB)
            for m in range(8):
                g = m // 2
                for kk in range(2):
                    E.mm(psA[:, 0:TW], pw[:, g, kk, (m % 2) * 128:(m % 2 + 1) * 128], pl[:, 2 * g + kk, 0:TW],
                         start=(kk == 0), stop=(kk == 1))
                E.stt(xt[:, m, 16:n], psA[:, 0:TW], gps[:, m:m + 1], xt[:, m, 16:n], MUL, ADD)
            E.dma(x3T[:, ti * TW:ti * TW + TW].rearrange("(c p) t -> p c t", p=128), xt[:, :, 16:n])


def kernel(**inputs):
    inp = {k: np.asarray(v) for k, v in inputs.items()}
    x = inp['x']
    B, L, _ = x.shape
    cfg = Cfg(L)
    nc, names = build(cfg)
    maps = prep_inputs(inp, cfg)
    maps = [{k: m[k] for k in names} for m in maps]
    res = run_bass_kernel_spmd(nc, maps, core_ids=list(range(8)))
    out = np.empty((B, L, D), np.float32)
    for core in range(8):
        b, s = core // 4, core % 4
        out[b, s * cfg.NOWN:(s + 1) * cfg.NOWN, :] = np.asarray(res.results[core]['outT']).T
    return out
```

```python
import numpy as np
import concourse.bass as bass
import concourse.mybir as mybir
from concourse.bass_utils import run_bass_kernel_spmd

F32 = mybir.dt.float32
BF16 = mybir.dt.bfloat16
I32 = mybir.dt.int32
AF = mybir.ActivationFunctionType
ALU = mybir.AluOpType
AX = mybir.AxisListType


class P:
    ENG = ('pe', 'act', 'dve', 'pool', 'sp')

    def __init__(self, nc):
        self.nc = nc
        self.ops = {e: [] for e in self.ENG}
        self.cnt = {e: 0 for e in self.ENG}
        self.csem = {e: nc.alloc_semaphore("c_" + e) for e in self.ENG}
        self.dsem = {}
        self.dcnt = {}
        self.last_w = {}
        self.readers = {}
        self.known = {e: {} for e in self.ENG}
        self.out_tokens = []
        self.n_sb = 0

    def sb(self, name, shape, dtype):
        return self.nc.alloc_sbuf_tensor("s_" + name, list(shape), dtype)

    def ps(self, name, shape, dtype=F32):
        return self.nc.alloc_psum_tensor(name, list(shape), dtype)

    def _deps(self, reads, writes):
        toks = []
        for k in reads:
            t = self.last_w.get(k)
            if t is not None:
                toks.append(t)
        for k in writes:
            t = self.last_w.get(k)
            if t is not None:
                toks.append(t)
            toks.extend(self.readers.get(k, ()))
        return toks

    def _waits(self, eng, toks):
        best = {}
        for (sem, val, peng) in toks:
            if peng == eng and eng in ('pe', 'sp'):
                continue
            key = id(sem)
            if val <= self.known[eng].get(key, 0):
                continue
            if key not in best or best[key][1] < val:
                best[key] = (sem, val)
        for key, (sem, val) in best.items():
            self.known[eng][key] = val
        return list(best.values())

    def _commit(self, tok, reads, writes):
        for k in writes:
            self.last_w[k] = tok
            self.readers[k] = []
        for k in reads:
            if k in writes:
                continue
            self.readers.setdefault(k, []).append(tok)

    def op(self, eng, fn, reads=(), writes=()):
        waits = self._waits(eng, self._deps(reads, writes))
        self.cnt[eng] += 1
        tok = (self.csem[eng], self.cnt[eng], eng)
        self.ops[eng].append((waits, fn, (self.csem[eng], 1)))
        self._commit(tok, reads, writes)
        return tok

    def dma(self, q, out_ap, in_ap, reads=(), writes=(), out=False, sem=None, **kw):
        waits = self._waits(q, self._deps(reads, writes))
        skey = sem if sem is not None else writes[0]
        if skey not in self.dsem:
            self.dsem[skey] = self.nc.alloc_semaphore("d_%d" % len(self.dsem))
            self.dcnt[skey] = 0
        self.dcnt[skey] += 16
        s = self.dsem[skey]
        tok = (s, self.dcnt[skey], None)
        self.ops[q].append((waits, lambda e: e.dma_start(out_ap, in_ap, **kw), (s, 16)))
        self._commit(tok, reads, writes)
        if out:
            self.out_tokens.append(tok)
        return tok

    def barrier(self):
        toks = [(self.csem[e], self.cnt[e], None) for e in self.ENG if self.cnt[e] > 0]
        toks += [(self.dsem[k], self.dcnt[k], None) for k in self.dsem]
        for e in self.ENG:
            w = self._waits(e, toks)
            if w:
                self.ops[e].append((w, None, None))

    def emit(self):
        nc = self.nc
        fin = self._waits('sp', self.out_tokens)
        ops = self.ops

        def replay(eng_name, e):
            for waits, fn, inc in ops[eng_name]:
                for (sem, val) in waits:
                    e.wait_ge(sem, val)
                if fn is None:
                    continue
                ins = fn(e)
                ins.then_inc(inc[0], inc[1])

        with nc.Block() as block:
            @block.tensor
            def _(e):
                replay('pe', e)

            @block.scalar
            def _(e):
                replay('act', e)

            @block.vector
            def _(e):
                replay('dve', e)

            @block.gpsimd
            def _(e):
                replay('pool', e)

            @block.sync
            def _(e):
                replay('sp', e)
                for (sem, val) in fin:
                    e.wait_ge(sem, val)


D = 1024
NCH = 8
P_IN = 2832
EPS = 1e-6
NEG = -30000.0


class Cfg:
    def __init__(self, seq):
        self.SEQ = seq
        self.NBO = seq // 128 // 4
        self.NBE = self.NBO + 1
        self.NBT = 4 * self.NBO
        self.NT = self.NBT * 128
        self.NOWN = self.NBO * 128
        self.NEXT = self.NBE * 128
        self.PRE = self.NBT - self.NBE
        assert self.NBT % 4 == 0


def _names(aps):
    out = []
    for a in aps:
        if a is None or isinstance(a, (int, float)):
            continue
        n = a.name
        if n not in out:
            out.append(n)
    return out


import os
USE_POOL = os.environ.get("MK_POOL", "0") == "1"


def _e(eng):
    if eng == 'pool' and not USE_POOL:
        return 'dve'
    return eng


class Emit:
    def __init__(self, p):
        self.p = p

    def mm(self, out, lhsT, rhs, start=True, stop=True):
        self.p.op('pe', lambda e: e.matmul(out, lhsT, rhs, start=start, stop=stop),
                  reads=_names([lhsT, rhs]), writes=_names([out]))

    def tr(self, out, in_, ident):
        self.p.op('pe', lambda e: e.transpose(out, in_, ident),
                  reads=_names([in_, ident]), writes=_names([out]))

    def act(self, out, in_, func, bias=None, scale=None, accum=None, eng='act'):
        kw = {}
        if bias is not None:
            kw['bias'] = bias
        if scale is not None:
            kw['scale'] = scale
        if accum is not None:
            kw['accum_out'] = accum
        self.p.op('act', lambda e: e.activation(out, in_, func, **kw),
                  reads=_names([in_, bias, scale]), writes=_names([out, accum]))

    def tt(self, out, in0, in1, op, eng='dve'):
        eng = _e(eng)
        self.p.op(eng, lambda e: e.tensor_tensor(out, in0, in1, op),
                  reads=_names([in0, in1]), writes=_names([out]))

    def ts(self, out, in0, s1, op0, s2=None, op1=None, eng='dve'):
        eng = _e(eng)
        if op1 is None:
            self.p.op(eng, lambda e: e.tensor_scalar(out, in0, s1, None, op0),
                      reads=_names([in0, s1]), writes=_names([out]))
        else:
            self.p.op(eng, lambda e: e.tensor_scalar(out, in0, s1, s2, op0, op1),
                      reads=_names([in0, s1, s2]), writes=_names([out]))

    def stt(self, out, in0, scalar, in1, op0, op1, eng='dve'):
        eng = _e(eng)
        self.p.op(eng, lambda e: e.scalar_tensor_tensor(out, in0, scalar, in1, op0, op1),
                  reads=_names([in0, scalar, in1]), writes=_names([out]))

    def copy(self, out, in_, eng='act'):
        eng = _e(eng)
        if eng == 'act':
            self.p.op('act', lambda e: e.copy(out, in_), reads=_names([in_]), writes=_names([out]))
        else:
            self.p.op(eng, lambda e: e.tensor_copy(out, in_), reads=_names([in_]), writes=_names([out]))

    def red(self, out, in_, op, axis=AX.X):
        self.p.op('dve', lambda e: e.tensor_reduce(out, in_, axis, op),
                  reads=_names([in_]), writes=_names([out]))

    def recip(self, out, in_):
        self.p.op('dve', lambda e: e.reciprocal(out, in_), reads=_names([in_]), writes=_names([out]))

    def memset(self, ap, val, eng='dve'):
        eng = _e(eng)
        self.p.op(eng, lambda e: e.memset(ap, val), reads=[], writes=_names([ap]))

    def dma(self, out, in_, q='sp', final=False):
        self.p.dma(q, out, in_, reads=_names([in_]), writes=_names([out]), out=final)


MUL, ADD, SUB, MAX = ALU.mult, ALU.add, ALU.subtract, ALU.max


def build(cfg, stop_after=None, dbg=()):
    nc = bass.Bass("TRN2", target_bir_lowering=False)
    p = P(nc)
    E = Emit(p)
    NT, NEXT, NOWN = cfg.NT, cfg.NEXT, cfg.NOWN

    in_names = []

    def din(name, shape):
        in_names.append(name)
        return nc.dram_tensor(name, list(shape), F32, kind="ExternalInput").ap()

    xsT = din("xsT", [D, NT])
    maskrow = din("maskrow", [1, NT])
    condT_d = din("condT", [128, 8])
    ada_w = din("ada_w", [2, D, 6 * D])
    ada_b2 = din("ada_b2", [128, 2, 48])
    nw_d = din("nw", [128, 5, 8])
    w_in = din("w_in", [D, P_IN])
    w_out = din("w_out", [D, D])
    convw_d = din("convw", [128, 12, 4])
    rows_d = din("rows", [128, 160])
    bias_d = din("bias_tab", [128, 8, 256])
    cmask_d = din("cmask", [128, 12])
    poolfix_d = din("poolfix", [128, 8, 16])
    pool_w = din("pool_w", [4, 256, 256])
    pscale_d = din("pscale", [128, 8])
    r1w = r2w = wgate = wup = wdown = None
    if stop_after not in ('prologue', 'mixer0'):
        r1w = din("r1w", [2, D, 4])
        r2w = din("r2w", [2, D, 32])
        wgate = din("moe_w_gate", [2, 32, D, 512])
        wup = din("moe_w_up", [2, 32, D, 512])
        wdown = din("moe_w_down", [2, 32, 512, D])
    consts_d = din("consts", [128, 5, 128])
    outT = nc.dram_tensor("outT", [D, NOWN], F32, kind="ExternalOutput").ap()
    dbg_kind = "ExternalOutput" if dbg else "Internal"
    x1T = nc.dram_tensor("x1T", [D, NEXT], F32, kind=dbg_kind).ap()
    x2T = nc.dram_tensor("x2T", [D, NEXT], F32, kind=dbg_kind).ap()
    x3T = nc.dram_tensor("x3T", [D, NOWN], F32, kind=dbg_kind).ap()
    dbg_cat = nc.dram_tensor("dbg_cat", [NEXT, D], F32, kind="ExternalOutput").ap() if dbg else None

    psA = p.ps("psA", [128, 512]); psB = p.ps("psB", [128, 512]); psC = p.ps("psC", [128, 512])
    psD = p.ps("psD", [128, 512]); psE = p.ps("psE", [128, 512]); psF = p.ps("psF", [128, 512])
    psG = p.ps("psG", [128, 512]); psT = p.ps("psT", [128, 512])
    psTb = psT[:].bitcast(BF16)

    cst = p.sb("cst", [128, 5, 128], F32)
    E.dma(cst[:], consts_d)
    ident_f = cst[:, 0, :]; UTf = cst[:, 1, :]; SLf = cst[:, 2, :]; mstrict = cst[:, 3, :]; mincl = cst[:, 4, :]
    ident_b = p.sb("ident_b", [128, 128], BF16)
    E.copy(ident_b[:], ident_f, eng='dve')
    ones_f = p.sb("ones_f", [128, 128], F32)
    E.memset(ones_f[:], 1.0)
    ones_b = p.sb("ones_b", [128, 128], BF16)
    E.memset(ones_b[:], 1.0)
    blk_b = p.sb("blk_b", [128, 128], BF16)
    E.memset(blk_b[:], 0.0)
    E.memset(blk_b[0:64, 0:64], 1.0)
    E.memset(blk_b[64:128, 64:128], 1.0)
    rows = p.sb("rows", [128, 160], F32)
    E.dma(rows[:], rows_d)
    nw = p.sb("nw", [128, 5, 8], F32)
    E.dma(nw[:], nw_d)
    cmask = p.sb("cmask", [128, 12], F32)
    E.dma(cmask[:], cmask_d)
    modT = p.sb("modT", [128, 2, 48], F32)
    adab = p.sb("adab", [128, 2, 48], F32)
    E.dma(adab[:], ada_b2)
    cond = p.sb("cond", [128, 8], F32)
    E.dma(cond[:], condT_d)
    E.act(cond[:], cond[:], AF.Silu)
    negA = p.sb("negA", [128, 8], F32)
    E.act(negA[:], rows[:, 0:8], AF.Exp)
    E.ts(negA[:], negA[:], -1.0, MUL)
    dtb = rows[:, 8:16]; onw = rows[:, 16:80]; sinks = rows[:, 80:88]

    with nc.sbuf_tensor("s_adaw0", [128, 8, 1024], F32) as aw0, nc.sbuf_tensor("s_adaw1", [128, 8, 1024], F32) as aw1:
        aws = [aw0, aw1]
        i = 0
        for l in range(2):
            for s in range(6):
                aw = aws[i % 2]; i += 1
                E.dma(aw[:], ada_w[l, :, s * 1024:(s + 1) * 1024].rearrange("(k p) n -> p k n", p=128))
                for j in range(8):
                    col = s * 8 + j
                    for k in range(8):
                        E.mm(psD[:, col:col + 1], aw[:, k, j * 128:(j + 1) * 128], cond[:, k:k + 1],
                             start=(k == 0), stop=(k == 7))
            E.tt(modT[:, l, :], psD[:, 0:48], adab[:, l, :], ADD)
        p.barrier()
    AB = p.sb("AB", [128, 2, 6, 8], F32)
    for l in range(2):
        for (dst, sc_i, nwi) in ((0, 1, 2 * l), (3, 4, 2 * l + 1)):
            E.ts(AB[:, l, dst, :], modT[:, l, sc_i * 8:(sc_i + 1) * 8], 1.0, ADD)
            E.tt(AB[:, l, dst, :], AB[:, l, dst, :], nw[:, nwi, :], MUL)
        E.copy(AB[:, l, 1, :], modT[:, l, 0:8], eng='dve')
        E.copy(AB[:, l, 2, :], modT[:, l, 16:24], eng='dve')
        E.copy(AB[:, l, 4, :], modT[:, l, 24:32], eng='dve')
        E.copy(AB[:, l, 5, :], modT[:, l, 40:48], eng='dve')

    ctx = dict(nc=nc, p=p, E=E, cfg=cfg, dbg_cat=dbg_cat, xsT=xsT, maskrow=maskrow, w_in=w_in, w_out=w_out, convw_d=convw_d,
               bias_d=bias_d, x1T=x1T, x2T=x2T, x3T=x3T, outT=outT, AB=AB, nw=nw, rows=rows, negA=negA,
               cmask=cmask, ident_f=ident_f, ident_b=ident_b, UTf=UTf, SLf=SLf, mstrict=mstrict, mincl=mincl,
               ones_f=ones_f, ones_b=ones_b, blk_b=blk_b, dtb=dtb, onw=onw, sinks=sinks,
               ps=dict(A=psA, B=psB, C=psC, D=psD, E=psE, F=psF, G=psG, T=psT, Tb=psTb),
               pool_w=pool_w, pscale_d=pscale_d, poolfix_d=poolfix_d, r1w=r1w, r2w=r2w,
               wgate=wgate, wup=wup, wdown=wdown, modT=modT)

    if stop_after != 'prologue':
        phase_mixer0(ctx)
        p.barrier()
    if stop_after not in ('prologue', 'mixer0'):
        phase_moe(ctx, 0, x1T, cfg.NBE, x2T, final=False)
        p.barrier()
    if stop_after not in ('prologue', 'mixer0', 'moe0'):
        phase_pool(ctx)
        p.barrier()
    if stop_after not in ('prologue', 'mixer0', 'moe0', 'pool'):
        phase_moe(ctx, 1, x3T, cfg.NBO, outT, final=True)
    if dbg:
        dm = nc.dram_tensor("dbg_mod", [128, 2, 48], F32, kind="ExternalOutput").ap()
        E.dma(dm, modT[:], final=True)
    p.emit()
    return nc, in_names


def bcl(ap, n):
    sh = list(ap.shape)
    return ap.unsqueeze(len(sh)).broadcast_to(sh + [n])


def bcm(ap, n):
    sh = list(ap.shape)
    return ap.unsqueeze(1).broadcast_to([sh[0], n] + sh[1:])


def v3(ap, h):
    return ap.rearrange("p (h j) -> p h j", h=h)


def rsqrt_act(E, out, in_, scale, eps):
    E.act(out, in_, AF.Ln, bias=eps, scale=scale)
    E.act(out, out, AF.Exp, scale=-0.5)


class StopPhase(Exception):
    pass


def cutpoint(n):
    import os
    c = os.environ.get("MK_CUT", "")
    if c and float("0." + str(n)) >= float("0." + c):
        raise StopPhase()


def phase_mixer0(ctx):
    try:
        _phase_mixer0(ctx)
    except StopPhase:
        pass


def _phase_mixer0(ctx):
    from contextlib import ExitStack
    nc, p, E, cfg = ctx['nc'], ctx['p'], ctx['E'], ctx['cfg']
    ps = ctx['ps']
    psA, psB, psC, psD, psE, psF, psG, psT, psTb = (ps[k] for k in ('A', 'B', 'C', 'D', 'E', 'F', 'G', 'T', 'Tb'))
    AB, rows, negA, cmask = ctx['AB'], ctx['rows'], ctx['negA'], ctx['cmask']
    ident_f, ident_b, UTf, SLf, mstrict, mincl = (ctx[k] for k in ('ident_f', 'ident_b', 'UTf', 'SLf', 'mstrict', 'mincl'))
    ones_f, blk_b, dtb, onw, sinks = (ctx[k] for k in ('ones_f', 'blk_b', 'dtb', 'onw', 'sinks'))
    xsT, maskrow, x1T = ctx['xsT'], ctx['maskrow'], ctx['x1T']
    A1, B1, G1 = AB[:, 0, 0, :], AB[:, 0, 1, :], AB[:, 0, 2, :]
    NTILE = cfg.NBT // 4
    PRE = cfg.PRE
    first_q_tile = PRE // 4
    first_kb_tile = max(0, (PRE - 1) // 4)
    R = 2
    CH = F32 if os.environ.get('MK_CHAIN_F32', '1') == '1' else BF16
    F32R = mybir.dt.float32r
    USE_R = CH == F32 and os.environ.get('MK_F32R', '0') == '1'

    def rr(ap):
        return ap.bitcast(F32R) if USE_R else ap
    ident_c = ident_f if CH == F32 else ident_b[:]
    psTc = psT[:] if CH == F32 else psTb
    TW = 0 if CH == F32 else 1

    print('mixer0 sbuf remaining', nc.sbuf_bytes_remaining)
    with ExitStack() as es:
        def sb(name, shape, dt):
            return es.enter_context(nc.sbuf_tensor("s_" + name, list(shape), dt))

        win = sb("win_sb", [128, 8, P_IN], BF16)
        EXP = os.environ.get('MK_EXP', '0') == '1'
        wout = sb("wout_sb", [128, 8, D] if not EXP else [128, 8, 128], BF16)
        for k in range(8):
            E.dma(win[:, k, :], ctx['w_in'][k * 128:(k + 1) * 128, :], q='pool')
        for k in range(8):
            if not EXP:
                E.dma(wout[:, k, :], ctx['w_out'][k * 128:(k + 1) * 128, :], q='pool')
        convw = sb("convw", [128, 12, 4], F32)
        E.dma(convw[:], ctx['convw_d'])
        biast = sb("biast", [128, 8, 256], BF16)
        E.dma(biast[:], ctx['bias_d'], q='pool')
        xts = [sb("xt0", [128, 8, 512], F32)] * 2
        mk = sb("mk", [128, 512], F32)
        rstd = sb("rstd", [128, 512], F32)
        hT = sb("hT", [128, 8, 512], BF16)
        raws = [sb("raw0", [128, 515], F32)] * 2
        carry = sb("carry", [128, 12, 3], F32)
        E.memset(carry[:], 0.0)
        cvs = [sb("cv%d" % i, [128, 512], F32) for i in range(2)]
        sls = [sb("sl0", [128, 512], F32)] * 2
        sqs = cvs
        tmpx = sls
        sqb = sb("sqb", [128, 512], BF16)
        rn = rstd
        kTn = sb("kTn", [128, 4, 512], BF16)
        qTn = sb("qTn", [128, 4, 512], BF16)
        kTm = [sb("kTm%d" % i, [128, 4, 128], BF16) for i in range(2)]
        vT = sb("vT", [128, 4, 512], BF16)
        qbT = sb("qbT", [128, 4, 512], BF16)
        kbT = sb("kbT", [128, 4, 640], BF16)
        wkb = sb("wkb", [128, 8, 2, 128], BF16)
        for kv_ in range(2):
            for dup_ in range(2):
                E.copy(wkb[:, :, kv_, dup_ * 64:(dup_ + 1) * 64], win[:, :, 2576 + kv_ * 64:2576 + (kv_ + 1) * 64], eng='dve')
        pm = cmask[:, 2:4]
        pm8 = cmask[:, 4:12]
        E.memset(kbT[:], 0.0)
        smTs = [sb("smT%d" % i, [128, 4, 64], F32) for i in range(2)]
        stT = sb("stT", [128, 4, 24], F32)
        smt = sb("smt", [128, 64], F32)
        G3 = [sb("G3_0", [128, 4, 128], F32)] * 2
        Ef = [sb("Ef_0", [128, 4, 128], F32)] * 2
        Dsb = [sb("Dsb_0", [128, 4, 128], F32)] * 2
        Di = [sb("Di_0", [128, 4, 128], F32)] * 2
        Nb = [[sb("N%d_%d" % (a, i), [128, 4, 128], BF16) for i in range(2)] for a in range(2)]
        NTb = [[sb("NT%d_%d" % (a, i), [128, 4, 128], BF16) for i in range(2)] for a in range(2)]
        Yb = [[sb("Y%d_%d" % (a, i), [128, 4, 128], BF16) for i in range(2)] for a in range(2)]
        Ai = [sb("Ai_%d" % i, [128, 4, 128], F32) for i in range(2)]
        Z0f = [sb("Z0f_%d" % i, [128, 4, 128], F32) for i in range(2)]
        Z0T = [sb("Z0T_%d" % i, [128, 4, 128], F32) for i in range(2)]
        T1buf = [G3[0], Ef[0]]
        aqk = [sb("aqk_%d" % i, [128, 4, 128], BF16) for i in range(2)]
        bv = sb("bv", [128, 8, 64], CH)
        kbe = sb("kbe", [128, 8, 2, 64], CH)
        u_r = [sb("u_%d" % i, [128, 8, 64], F32) for i in range(R)]
        wT_r = [sb("wT_%d" % i, [128, 8, 128], BF16) for i in range(R)]
        kdd_r = [sb("kdd_%d" % i, [128, 8, 2, 64], BF16) for i in range(R)]
        aqkT_r = [sb("aqkT_%d" % i, [128, 8, 128], BF16) for i in range(R)]
        vb_r = [sb("vb_%d" % i, [128, 128], BF16) for i in range(R)]
        for i in range(R):
            E.memset(vb_r[i][:], 0.0)
        S = sb("S", [128, 8, 64], F32)
        S_tmp = sb("S_tmp", [128, 8, 64], F32)
        S_bf = sb("S_bf", [128, 8, 64], BF16)
        E.memset(S[:], 0.0)
        E.memset(S_bf[:], 0.0)
        vnew = sb("vnew", [128, 8, 64], BF16)
        o2s = sb("o2s", [128, 8, 64], F32)
        o_a = sb("o_a", [128, 8, 64], F32)
        osq = o2s
        z_s = o2s
        cat = sb("cat", [128, 16, 64], BF16)
        catT = sb("catT", [128, 8, 128], BF16)
        scs = [o2s[:].rearrange("p a b -> p (a b)").rearrange("p (a b) -> p a b", a=2)] * 2
        p_bf = sqb[:].rearrange("p (a b) -> p a b", a=2)
        pT = vnew[:].rearrange("p a (c d) -> p (a c) d", c=1).rearrange("p (a b) d -> p a (b d)", b=2)
        den = sb("den", [128, 16], F32)

        for ti in range(NTILE):
            xt = xts[ti % 2]
            c0 = ti * 512
            need_q = ti >= first_q_tile
            need_kb = ti >= first_kb_tile
            E.dma(xt[:], xsT[:, c0:c0 + 512].rearrange("(c p) t -> p c t", p=128))
            E.dma(mk[:], maskrow[0, c0:c0 + 512].partition_broadcast(128))
            for c in range(8):
                sq = sqs[c % 2]
                E.act(sq[:], xt[:, c, :], AF.Square)
                E.mm(psD[:], ones_f[:], sq[:], start=(c == 0), stop=(c == 7))
            rsqrt_act(E, rstd[:], psD[:], 1.0 / D, EPS)
            for c in range(8):
                tx = tmpx[c % 2]
                E.tt(tx[:], xt[:, c, :], rstd[:], MUL)
                E.act(tx[:], tx[:], AF.Identity, bias=B1[:, c:c + 1], scale=A1[:, c:c + 1])
                E.tt(hT[:, c, :], tx[:], mk[:], MUL, eng='pool')
            cutpoint(1)
            chunks = list(range(12)) if need_q else list(range(4, 12))
            for ci, cc in enumerate(chunks):
                raw = raws[ci % 2]
                for k in range(8):
                    E.mm(psD[:], win[:, k, cc * 128:(cc + 1) * 128], hT[:, k, :], start=(k == 0), stop=(k == 7))
                E.copy(raw[:, 0:3], carry[:, cc, :], eng='pool')
                E.copy(raw[:, 3:515], psD[:])
                E.copy(carry[:, cc, :], raw[:, 512:515], eng='pool')
                cv = cvs[ci % 2]
                E.ts(cv[:], raw[:, 0:512], convw[:, cc, 0:1], MUL)
                for j in range(1, 4):
                    E.stt(cv[:], raw[:, j:j + 512], convw[:, cc, j:j + 1], cv[:], MUL, ADD)
                if cc >= 8:
                    E.act(vT[:, cc - 8, :], cv[:], AF.Silu)
                else:
                    sl = sls[ci % 2]
                    E.act(sl[:], cv[:], AF.Silu)
                    E.act(sqb[:], sl[:], AF.Square)
                    E.mm(psE[:], blk_b[:], sqb[:])
                    rsqrt_act(E, rn[:], psE[:], 1.0, EPS)
                    if cc < 4:
                        E.stt(qTn[:, cc, :], sl[:], 0.125, rn[:], MUL, MUL)
                    else:
                        E.tt(kTn[:, cc - 4, :], sl[:], rn[:], MUL)
            if need_q:
                for c in range(4):
                    for k in range(8):
                        E.mm(psD[:], win[:, k, 2064 + c * 128:2064 + (c + 1) * 128], hT[:, k, :],
                             start=(k == 0), stop=(k == 7))
                    E.copy(qbT[:, c, :], psD[:])
            if need_kb:
                for kv in range(2):
                    for k in range(8):
                        E.mm(psD[:], wkb[:, k, kv, :], hT[:, k, :], start=(k == 0), stop=(k == 7))
                    for par in range(2):
                        E.ts(kbT[:, kv * 2 + par, 128:640], psD[:], pm[:, par:par + 1], MUL)

            cutpoint(2)
            smT = smTs[ti % 2]
            BA = psG[:, 0:128].rearrange("p (b c) -> p b c", b=4)
            for bi in range(4):
                for k in range(8):
                    E.mm(psG[:, bi * 32:bi * 32 + 16], hT[:, k, bi * 128:(bi + 1) * 128], win[:, k, 2048:2064],
                         start=(k == 0), stop=(k == 7))
            gT, betaT, egcT, ekdT, egtT, begeT, gcsT = (smT[:, :, i * 8:(i + 1) * 8] for i in range(7))
            E.act(stT[:, :, 0:8], BA[:, :, 0:8], AF.Exp, scale=-1.0)
            E.ts(stT[:, :, 0:8], stT[:, :, 0:8], 1.0, ADD)
            E.recip(betaT, stT[:, :, 0:8])
            E.tt(stT[:, :, 8:16], BA[:, :, 8:16], bcm(dtb, 4), ADD)
            E.act(stT[:, :, 8:16], stT[:, :, 8:16], AF.Exp)
            E.act(stT[:, :, 8:16], stT[:, :, 8:16], AF.Ln, bias=1.0)
            E.tt(gT, stT[:, :, 8:16], bcm(negA[:], 4), MUL)
            for bi in range(4):
                E.mm(psG[:, bi * 32 + 16:bi * 32 + 24], UTf, smT[:, bi, 0:8])
                E.mm(psG[:, bi * 32 + 24:bi * 32 + 32], ones_f[:], smT[:, bi, 0:8])
            E.copy(gcsT, BA[:, :, 16:24])
            E.act(egcT, BA[:, :, 16:24], AF.Exp)
            E.act(egtT, BA[:, :, 24:32], AF.Exp)
            E.tt(stT[:, :, 16:24], BA[:, :, 24:32], gcsT, SUB)
            E.act(ekdT, stT[:, :, 16:24], AF.Exp)
            E.tt(begeT, betaT, egcT, MUL)

            def blk_pre(bi):
                    b = ti * 4 + bi
                    t0 = bi * 128
                    own = b >= PRE
                    r = b % R
                    g, beta, egc, ekd, egt, bege, gcs = (smT[:, bi, i * 8:(i + 1) * 8] for i in range(7))
                    kdd = kdd_r[r]

                    for c in range(4):
                        E.tr(psTb[:, c * 128:(c + 1) * 128], vT[:, c, t0:t0 + 128], ident_b[:])
                    for c in range(4):
                        E.tr(psTb[:, 512 + c * 128:512 + (c + 1) * 128], kTn[:, c, t0:t0 + 128], ident_b[:])
                    vtok = v3(psTb[:, 0:512], 8)
                    ktok = v3(psTb[:, 512:1024], 8)
                    E.tt(bv[:], vtok, bcl(beta, 64), MUL)
                    E.tt(kbe[:, :, 0, :], ktok, bcl(bege, 64), MUL)
                    E.copy(kbe[:, :, 1, :], kbe[:, :, 0, :], eng='pool')
                    kdd = kdd_r[r]
                    E.tt(kdd[:, :, 0, :], ktok, bcl(ekd, 64), MUL)
                    E.copy(kdd[:, :, 1, :], kdd[:, :, 0, :], eng='pool')
                    yield
                    for par in range(2):
                        E.ts(kTm[par][:], kTn[:, :, t0:t0 + 128], pm[:, par:par + 1], MUL)
                    for hh in range(2):
                        h0 = hh * 4
                        E.tt(G3[hh][:], bcm(UTf, 4), bcl(g[:, h0:h0 + 4], 128), MUL, eng='pool')
                        for j in range(4):
                            E.mm(psA[:, j * 128:(j + 1) * 128], G3[hh][:, j, :], SLf)
                        E.act(Ef[hh][:], v3(psA[:], 4), AF.Exp)
                        E.tt(Dsb[hh][:], Ef[hh][:], bcm(mstrict, 4), MUL, eng='pool')
                        E.tt(Dsb[hh][:], Dsb[hh][:], bcl(beta[:, h0:h0 + 4], 128), MUL, eng='pool')
                        for j in range(4):
                            h = h0 + j
                            c, r0 = h // 2, (h % 2) * 64
                            E.mm(psB[:, j * 128:(j + 1) * 128], kTn[:, c, t0:t0 + 128], kTm[h % 2][:, c, :])
                        yield
                        N0f = Ai[hh]
                        E.tt(N0f[:], v3(psB[:], 4), Dsb[hh][:], MUL)
                        N0 = Nb[0][hh]
                        E.copy(N0[:], N0f[:])
                        E.tt(N0f[:], N0f[:], bcm(ident_f, 4), ADD)
                        if own:
                            E.tt(Di[hh][:], Ef[hh][:], bcm(mincl, 4), MUL, eng='pool')
                            for j in range(4):
                                h = h0 + j
                                c, r0 = h // 2, (h % 2) * 64
                                E.mm(psC[:, j * 128:(j + 1) * 128], qTn[:, c, t0:t0 + 128], kTm[h % 2][:, c, :])
                            E.tt(aqk[hh][:], v3(psC[:], 4), Di[hh][:], MUL)
                            for j in range(4):
                                E.tr(psTb[:, hh * 512 + j * 128:hh * 512 + (j + 1) * 128], aqk[hh][:, j, :], ident_b[:])
                            E.copy(aqkT_r[r][:, h0:h0 + 4, :], v3(psTb[:, hh * 512:(hh + 1) * 512], 4))
                        for j in range(4):
                            E.tr(psTb[:, hh * 512 + j * 128:hh * 512 + (j + 1) * 128], N0[:, j, :], ident_b[:])
                        ntp = v3(psTb[:, hh * 512:(hh + 1) * 512], 4)
                        NT0 = NTb[0][hh]
                        E.copy(NT0[:], ntp)
                        Y0 = Yb[0][hh]
                        E.ts(Y0[:], NT0[:], -1.0, MUL)
                        E.tt(Y0[:], Y0[:], bcm(ident_f, 4), ADD)
                        yield

            def blk_chain(bi):
                    b = ti * 4 + bi
                    t0 = bi * 128
                    own = b >= PRE
                    r = b % R
                    g, beta, egc, ekd, egt, bege, gcs = (smT[:, bi, i * 8:(i + 1) * 8] for i in range(7))
                    kdd = kdd_r[r]

                    PA, PB, PC = (psA, psE), (psB, psF), (psC, psG)
                    cur = 0
                    for lvl in range(1, 7):
                        for hh in range(2):
                            Nc, NTc = Nb[cur][hh], NTb[cur][hh]
                            for j in range(4):
                                E.mm(PA[hh][:, j * 128:(j + 1) * 128], NTc[:, j, :], Nc[:, j, :])
                            if lvl < 6:
                                for j in range(4):
                                    E.mm(PB[hh][:, j * 128:(j + 1) * 128], Nc[:, j, :], NTc[:, j, :])
                        for hh in range(2):
                            E.copy(Nb[1 - cur][hh][:], v3(PA[hh][:], 4))
                            if lvl < 6:
                                E.copy(NTb[1 - cur][hh][:], v3(PB[hh][:], 4), eng='dve')
                        for hh in range(2):
                            Nn, Yc = Nb[1 - cur][hh], Yb[cur][hh]
                            for j in range(4):
                                E.mm(PC[hh][:, j * 128:(j + 1) * 128], Nn[:, j, :], Yc[:, j, :])
                        for hh in range(2):
                            E.tt(Yb[1 - cur][hh][:], v3(PC[hh][:], 4), Yb[cur][hh][:], ADD)
                        cur = 1 - cur
                    NEWTON = int(os.environ.get('MK_NEWTON', '1'))
                    for hh in range(2):
                        E.copy(Z0f[hh][:], Yb[cur][hh][:])
                    for it in range(NEWTON):
                        for hh in range(2):
                            for j in range(4):
                                E.mm(PA[hh][:, j * 128:(j + 1) * 128], Ai[hh][:, j, :], Z0f[hh][:, j, :])
                            for j in range(4):
                                E.tr(PB[hh][:, j * 128:(j + 1) * 128], Z0f[hh][:, j, :], ident_f)
                        for hh in range(2):
                            E.copy(Z0T[hh][:], v3(PB[hh][:], 4), eng='dve')
                        T1s = Yb16f = None
                        for hh in range(2):
                            E.copy(T1buf[hh][:], v3(PA[hh][:], 4))
                        for hh in range(2):
                            for j in range(4):
                                E.mm(PC[hh][:, j * 128:(j + 1) * 128], Z0T[hh][:, j, :], T1buf[hh][:, j, :])
                        for hh in range(2):
                            E.ts(Z0f[hh][:], Z0f[hh][:], 2.0, MUL)
                            E.tt(Z0f[hh][:], Z0f[hh][:], v3(PC[hh][:], 4), SUB)
                    for hh in range(2):
                        h0 = hh * 4
                        Yf = Z0f[hh]
                        for j in range(4):
                            h = h0 + j
                            E.mm(psD[:, h * 64:(h + 1) * 64], rr(Yf[:, j, :]), rr(bv[:, h, :]))
                        for j in range(4):
                            h = h0 + j
                            E.mm(PC[hh][:, j * 128:(j + 1) * 128], rr(kbe[:, h, :, :].rearrange("p a b -> p (a b)")), rr(Yf[:, j, :]))
                        E.copy(wT_r[r][:, h0:h0 + 4, :], v3(PC[hh][:], 4))
                    E.copy(u_r[r][:], v3(psD[:], 8))

            def blk_rec(bi):
                    b = ti * 4 + bi
                    t0 = bi * 128
                    own = b >= PRE
                    r = b % R
                    g, beta, egc, ekd, egt, bege, gcs = (smT[:, bi, i * 8:(i + 1) * 8] for i in range(7))
                    kdd = kdd_r[r]

                    for h in range(8):
                        E.mm(psE[:, h * 64:(h + 1) * 64], wT_r[r][:, h, :], S_bf[:, h, :])
                    E.tt(vnew[:], u_r[r][:], v3(psE[:], 8), SUB)
                    yield
                    if own:
                        for h in range(8):
                            c, r0 = h // 2, (h % 2) * 64
                            E.mm(psF[:, h * 64:(h + 1) * 64], qTn[:, c, t0:t0 + 128], S_bf[:, h, :])
                        for h in range(8):
                            E.mm(psG[:, h * 64:(h + 1) * 64], aqkT_r[r][:, h, :], vnew[:, h, :])
                    E.tt(S_tmp[:], S[:], bcl(egt, 64), MUL, eng='pool')
                    for h in range(8):
                        E.mm(psE[:, h * 64:(h + 1) * 64], kdd[:, h, :, :].rearrange("p a b -> p (a b)"), vnew[:, h, :])
                    yield
                    if own:
                        E.copy(o2s[:], v3(psG[:], 8))
                        E.tt(o_a[:], v3(psF[:], 8), bcl(egc, 64), MUL)
                        E.tt(o_a[:], o_a[:], o2s[:], ADD, eng='pool')
                    E.tt(S[:], S_tmp[:], v3(psE[:], 8), ADD)
                    E.tt(S_bf[:], S[:], bcl(pm8, 64), MUL)
                    yield
                    if b >= PRE - 1:
                        for k in range(8):
                            E.mm(psG[:, 0:128], hT[:, k, t0:t0 + 128], win[:, k, 2704:2832], start=(k == 0), stop=(k == 7))
                        E.copy(vb_r[r][:], psG[:, 0:128])
                    if not own:
                        return
                    E.tt(osq[:], o_a[:], o_a[:], MUL, eng='pool')
                    E.red(den[:, 8:16], osq[:], ADD)
                    rsqrt_act(E, den[:, 8:16], den[:, 8:16], 1.0 / 64, EPS)
                    E.tt(o_a[:], o_a[:], bcl(den[:, 8:16], 64), MUL)
                    E.tt(o_a[:], o_a[:], bcm(onw, 8), MUL, eng='pool')
                    for k in range(8):
                        E.mm(psF[:], hT[:, k, t0:t0 + 128], win[:, k, 1536:2048], start=(k == 0), stop=(k == 7))
                    E.act(z_s[:], v3(psF[:], 8), AF.Silu)
                    E.tt(cat[:, 0:8, :], o_a[:], z_s[:], MUL)
                    yield
                    vprev, vcur = vb_r[(b - 1) % R], vb_r[r]
                    for gi in range(4):
                        sc = scs[gi % 2]
                        for hh in range(2):
                            h = gi * 2 + hh
                            kv = h // 4
                            E.mm(psF[:, hh * 256:(hh + 1) * 256], qbT[:, h // 2, t0:t0 + 128], kbT[:, kv * 2 + h % 2, t0:t0 + 256])
                        E.stt(sc[:], v3(psF[:], 2), 0.125, biast[:, gi * 2:gi * 2 + 2, :], MUL, ADD)
                        if b == PRE + 1:
                            E.ts(sc[:, :, 0:128], sc[:, :, 0:128], cmask[:, 0:1], ADD)
                        mx = smt[:, 32 + gi * 2:32 + gi * 2 + 2]
                        E.red(mx, sc[:], MAX)
                        E.tt(mx, mx, sinks[:, gi * 2:gi * 2 + 2], MAX)
                        nmx = smt[:, 40 + gi * 2:40 + gi * 2 + 2]
                        E.ts(nmx, mx, -1.0, MUL)
                        for hh in range(2):
                            h = gi * 2 + hh
                            E.act(p_bf[:, hh, :], sc[:, hh, :], AF.Exp, bias=nmx[:, hh:hh + 1], accum=den[:, h:h + 1])
                        for hh in range(2):
                            for kb in range(2):
                                E.tr(psTb[:, (hh * 2 + kb) * 128:(hh * 2 + kb + 1) * 128], p_bf[:, hh, kb * 128:(kb + 1) * 128],
                                     ident_b[:])
                        E.copy(pT[:], v3(psTb[:, 0:512], 4))
                        yield
                        for hh in range(2):
                            h = gi * 2 + hh
                            kv = h // 4
                            E.mm(psE[:, h * 64:(h + 1) * 64], pT[:, hh * 2, :], vprev[:, kv * 64:(kv + 1) * 64],
                                 start=True, stop=False)
                            E.mm(psE[:, h * 64:(h + 1) * 64], pT[:, hh * 2 + 1, :], vcur[:, kv * 64:(kv + 1) * 64],
                                 start=False, stop=True)
                    E.tt(smt[:, 48:56], sinks, smt[:, 32:40], SUB)
                    E.act(smt[:, 48:56], smt[:, 48:56], AF.Exp)
                    E.tt(den[:, 0:8], den[:, 0:8], smt[:, 48:56], ADD)
                    E.recip(den[:, 0:8], den[:, 0:8])
                    E.tt(cat[:, 8:16, :], v3(psE[:], 8), bcl(den[:, 0:8], 64), MUL)
                    if ctx['dbg_cat'] is not None:
                        E.dma(ctx['dbg_cat'][(b - PRE) * 128:(b - PRE + 1) * 128, :], cat[:].rearrange("p a b -> p (a b)"), q='pool')
                    catf = cat[:].rearrange("p a b -> p (a b)")
                    for k in range(8):
                        E.tr(psTb[:, k * 128:(k + 1) * 128], catf[:, k * 128:(k + 1) * 128], ident_b[:])
                    E.copy(catT[:], v3(psTb[:, 0:1024], 8))
                    if not EXP:
                        for m in range(8):
                            for k in range(8):
                                E.mm(psD[:, 0:128], wout[:, k, m * 128:(m + 1) * 128], catT[:, k, :],
                                     start=(k == 0), stop=(k == 7))
                            E.stt(xt[:, m, t0:t0 + 128], psD[:, 0:128], G1[:, m:m + 1], xt[:, m, t0:t0 + 128], MUL, ADD)
                    yield

            def run_all(g):
                for _ in g:
                    pass

            def interleave(g1, g2):
                d1 = d2 = False
                while not (d1 and d2):
                    if not d1:
                        try:
                            next(g1)
                        except StopIteration:
                            d1 = True
                    if not d2:
                        try:
                            next(g2)
                        except StopIteration:
                            d2 = True

            PIPE = os.environ.get('MK_PIPE', '1') == '1'
            run_all(blk_pre(0))
            for bi in range(4):
                blk_chain(bi)
                if bi < 3 and PIPE:
                    interleave(blk_rec(bi), blk_pre(bi + 1))
                else:
                    run_all(blk_rec(bi))
                    if bi < 3:
                        run_all(blk_pre(bi + 1))

            if need_kb:
                E.copy(kbT[:, :, 0:128], kbT[:, :, 512:640], eng='pool')
            lo_b = max(PRE, ti * 4)
            if lo_b < ti * 4 + 4 and not EXP:
                lo = (lo_b - ti * 4) * 128
                n = 512 - lo
                e0 = (lo_b - PRE) * 128
                E.dma(x1T[:, e0:e0 + n].rearrange("(c p) t -> p c t", p=128), xt[:, :, lo:512])


def _t5_bucket_table():
    import math
    qi = np.arange(128)[:, None]
    ki = np.arange(256)[None, :]
    dist = qi + 128 - ki
    n = np.maximum(dist, 0)
    nf = np.maximum(n, 1).astype(np.float32)
    large = 16 + (np.log(nf / 16) / np.float32(math.log(128 / 16)) * 16).astype(np.int32)
    large = np.minimum(large, 31)
    bucket = np.where(n < 16, n, large)
    valid = (dist >= 0) & (dist < 128)
    return bucket, valid


def _static_consts():
    i = np.arange(128)
    same = np.ones((128, 128), bool)
    c = np.zeros((128, 5, 128), np.float32)
    c[:, 0, :] = np.eye(128)
    c[:, 1, :] = (i[:, None] <= i[None, :])
    c[:, 2, :] = (i[:, None] > i[None, :])
    c[:, 3, :] = (i[:, None] > i[None, :])
    c[:, 4, :] = (i[:, None] >= i[None, :])
    return c


def fm(v):
    v = np.asarray(v, np.float32)
    return np.ascontiguousarray(v.reshape(-1, 128).T)


def prep_inputs(inp, cfg):
    f32 = np.float32
    x = np.asarray(inp['x'], f32)
    B, L, _ = x.shape
    NT, NOWN = cfg.NT, cfg.NOWN
    bucket, valid = _t5_bucket_table()
    rel_bias = np.asarray(inp['rel_bias'], f32)
    bt = rel_bias[bucket]
    bt = np.where(valid[:, :, None], bt, f32(NEG))
    bias_tab = np.ascontiguousarray(bt.transpose(0, 2, 1))
    consts = _static_consts()
    rows = np.concatenate([inp['ab_a_log'][0], inp['ab_dt_bias'][0], inp['ab_onorm_w'][0], inp['ab_sinks'][0],
                           inp['r1_b'][0], inp['r2_b'][0], inp['r1_b'][1], inp['r2_b'][1]]).astype(f32)
    rows = np.ascontiguousarray(np.broadcast_to(rows[None, :], (128, rows.shape[0])))
    nw = np.stack([fm(inp['norm_mix_w'][0]), fm(inp['norm_ffn_w'][0]), fm(inp['norm_mix_w'][1]),
                   fm(inp['norm_ffn_w'][1]), fm(inp['final_norm_w'])], axis=1)
    ada_b2 = np.stack([fm(inp['ada_b'][0]), fm(inp['ada_b'][1])], axis=1)
    convw = np.ascontiguousarray(np.asarray(inp['ab_conv_w'][0], f32).T.reshape(12, 128, 4).transpose(1, 0, 2))
    pscale = fm(inp['pool_scale'][0])
    shared = dict(
        ada_w=np.asarray(inp['ada_w'], f32), ada_b2=ada_b2, nw=nw, w_in=np.asarray(inp['ab_w_in'][0], f32),
        w_out=np.asarray(inp['ab_w_out'][0], f32), convw=convw, rows=rows, bias_tab=bias_tab,
        pool_w=np.asarray(inp['pool_w'][0], f32), pscale=pscale, r1w=np.asarray(inp['r1_w'], f32),
        r2w=np.asarray(inp['r2_w'], f32), moe_w_gate=np.asarray(inp['moe_w_gate'], f32),
        moe_w_up=np.asarray(inp['moe_w_up'], f32), moe_w_down=np.asarray(inp['moe_w_down'], f32), consts=consts)
    maps = []
    wins = (2, 2, 4, 4, 8, 8, 16, 16)
    for core in range(8):
        b, s = core // 4, core % 4
        nreal = (s + 1) * NOWN
        xsT = np.zeros((D, NT), f32)
        xsT[:, NT - nreal:] = x[b, :nreal, :].T
        maskrow = np.zeros((1, NT), f32)
        maskrow[0, NT - nreal:] = 1.0
        cmask = np.zeros((128, 12), f32)
        cmask[:64, 2] = 1.0
        cmask[64:, 3] = 1.0
        for hh_ in range(8):
            cmask[:, 4 + hh_] = cmask[:, 2 + hh_ % 2]
        cmask[:, 0] = NEG if s == 0 else 0.0
        cmask[:, 1] = 0.0 if s == 0 else 1.0
        poolfix = np.zeros((128, 8, 16), f32)
        for c in range(8):
            w = wins[c]
            t = np.arange(16)
            cnt = np.minimum(t + 1, w) if s == 0 else np.full(16, w)
            poolfix[:, c, :] = (1.0 / cnt.astype(f32))[None, :]
        m = dict(shared)
        m.update(xsT=xsT, maskrow=maskrow, condT=fm(inp['c'][b]), cmask=cmask, poolfix=poolfix)
        maps.append(m)
    return maps


def phase_moe(ctx, layer, src, nblk, dst, final):
    from contextlib import ExitStack
    nc, p, E, cfg = ctx['nc'], ctx['p'], ctx['E'], ctx['cfg']
    ps = ctx['ps']
    psA, psB, psC, psD, psE, psF, psG, psT = (ps[k] for k in ('A', 'B', 'C', 'D', 'E', 'F', 'G', 'T'))
    AB, rows, nw = ctx['AB'], ctx['rows'], ctx['nw']
    ident_f, ones_f = ctx['ident_f'], ctx['ones_f']
    A2, B2, G2 = AB[:, layer, 3, :], AB[:, layer, 4, :], AB[:, layer, 5, :]
    r1b = rows[:, 88:92] if layer == 0 else rows[:, 124:128]
    r2b = rows[:, 92:124] if layer == 0 else rows[:, 128:160]
    wg_d, wu_d, wd_d = ctx['wgate'], ctx['wup'], ctx['wdown']
    npass = (nblk + 16) // 17
    per = (nblk + npass - 1) // npass
    passes = [list(range(i, min(i + per, nblk))) for i in range(0, nblk, per)]
    PB = max(len(x) for x in passes)

    with ExitStack() as es:
        def sb(name, shape, dt):
            return es.enter_context(nc.sbuf_tensor("s_m%d_" % layer + name, list(shape), dt))

        rw = sb("rw", [128, 8, 36], F32)
        E.dma(rw[:, :, 0:4], ctx['r1w'][layer].rearrange("(k p) n -> p k n", p=128))
        E.dma(rw[:, :, 4:36], ctx['r2w'][layer].rearrange("(k p) n -> p k n", p=128))
        h2 = sb("h2", [128, 8, PB * 128], BF16)
        acc = sb("acc", [128, PB, D], F32)
        Gt = sb("Gt", [128, PB, 32], F32)
        xt = sb("xt", [128, 8, 512], F32)
        stg = [sb("stg%d" % i, [128, 2, 512], F32) for i in range(4)]

        def hfc(c):
            return stg[c // 2][:, c % 2, :]
        sq = sb("sq", [128, 512], F32)
        rstd = sb("rstd", [128, 512], F32)
        wgs = [sb("wg%d" % i, [128, 8, 512], BF16) for i in range(2)]
        wus = [sb("wu%d" % i, [128, 8, 512], BF16) for i in range(2)]
        wds = [sb("wd%d" % i, [128, 4, D], BF16) for i in range(2)]
        sg = [sb("sg%d" % i, [128, 512], F32) for i in range(2)]
        he = sb("he", [128, 4, 512], BF16)
        rtT = sb("rtT", [128, 4, 128], F32)
        qT = sb("qT", [128, 4, 64], F32)

        class Loader:
            def __init__(self):
                self.n = 0

            def items(self, e, slot):
                it = []
                for kp in range(4):
                    it.append((wgs[slot][:, 2 * kp:2 * kp + 2, :],
                               wg_d[layer, e][kp * 256:(kp + 1) * 256, :].rearrange("(k p) n -> p k n", p=128)))
                for kp in range(4):
                    it.append((wus[slot][:, 2 * kp:2 * kp + 2, :],
                               wu_d[layer, e][kp * 256:(kp + 1) * 256, :].rearrange("(k p) n -> p k n", p=128)))
                for k in range(4):
                    it.append((wds[slot][:, k, :].rearrange("p (a b) -> p a b", a=2),
                               wd_d[layer, e][k * 128:(k + 1) * 128, :].rearrange("p (a b) -> p a b", a=2)))
                return it

            def start(self, e, slot):
                self.it = self.items(e, slot)
                self.di = 0
                self.ci = 0
                self.base = self.n
                for _ in range(3):
                    self.dma_next()

            def dma_next(self):
                if self.di < len(self.it):
                    st = stg[(self.base + self.di) % 4]
                    E.dma(st[:], self.it[self.di][1])
                    self.di += 1

            def step(self):
                if self.ci < len(self.it):
                    self.dma_next()
                    st = stg[(self.base + self.ci) % 4]
                    E.copy(self.it[self.ci][0], st[:])
                    self.ci += 1
                    self.n += 1
                    return True
                return False

            def flush(self):
                while self.step():
                    pass

        ld = Loader()

        for blocks in passes:
            nb = len(blocks)
            tiles = [blocks[i:i + 4] for i in range(0, nb, 4)]
            for tl in tiles:
                n = len(tl) * 128
                c0 = tl[0] * 128
                l0 = (tl[0] - blocks[0]) * 128
                E.dma(xt[:, :, 0:n], src[:, c0:c0 + n].rearrange("(c p) t -> p c t", p=128))
                for c in range(8):
                    E.act(sq[:, 0:n], xt[:, c, 0:n], AF.Square)
                    E.mm(psD[:, 0:n], ones_f[:], sq[:, 0:n], start=(c == 0), stop=(c == 7))
                rsqrt_act(E, rstd[:, 0:n], psD[:, 0:n], 1.0 / D, EPS)
                for c in range(8):
                    E.tt(hfc(c)[:, 0:n], xt[:, c, 0:n], rstd[:, 0:n], MUL)
                    E.act(hfc(c)[:, 0:n], hfc(c)[:, 0:n], AF.Identity, bias=B2[:, c:c + 1], scale=A2[:, c:c + 1])
                    E.copy(h2[:, c, l0:l0 + n], hfc(c)[:, 0:n], eng='dve')
                nbt = len(tl)
                lb0 = tl[0] - blocks[0]
                for bi in range(nbt):
                    for k in range(8):
                        E.mm(psE[:, bi * 36:(bi + 1) * 36], hfc(k)[:, bi * 128:(bi + 1) * 128], rw[:, k, :],
                             start=(k == 0), stop=(k == 7))
                L = psE[:, 0:nbt * 36].rearrange("p (b c) -> p b c", b=nbt)
                Rr = rtT[:, 0:nbt, :]
                Qq = qT[:, 0:nbt, :]
                l1 = Rr[:, :, 0:4]; l2 = Rr[:, :, 4:36]
                E.tt(l1, L[:, :, 0:4], bcm(r1b, nbt), ADD)
                E.tt(l2, L[:, :, 4:36], bcm(r2b, nbt), ADD)
                m1 = Rr[:, :, 36]
                E.red(m1, l1, MAX)
                oh = Rr[:, :, 40:44]
                E.tt(oh, l1, bcl(m1, 4), ALU.is_equal)
                ex = Rr[:, :, 44:48]
                E.tt(ex, l1, bcl(m1, 4), SUB)
                E.act(ex, ex, AF.Exp)
                ptop = Rr[:, :, 39]
                E.red(ptop, ex, ADD)
                E.recip(ptop, ptop)
                pen = Rr[:, :, 48:52]
                E.ts(pen, oh, 1.0, SUB, 1.0e30, MUL)
                lm = Rr[:, :, 52:84]
                E.tt(lm.rearrange("p b (g e) -> p b g e", g=4), l2.rearrange("p b (g e) -> p b g e", g=4),
                     pen.unsqueeze(3).broadcast_to([128, nbt, 4, 8]), ADD)
                t1 = Rr[:, :, 84]
                E.red(t1, lm, MAX)
                o1 = Rr[:, :, 88:120]
                E.tt(o1, lm, bcl(t1, 32), ALU.is_equal)
                lm2 = Qq[:, :, 0:32]
                E.stt(lm2, o1, -1.0e30, lm, MUL, ADD)
                t2 = Rr[:, :, 85]
                E.red(t2, lm2, MAX)
                o2 = Qq[:, :, 32:64]
                E.tt(o2, lm2, bcl(t2, 32), ALU.is_equal)
                dd = Rr[:, :, 86]
                E.tt(dd, t2, t1, SUB)
                E.act(dd, dd, AF.Exp)
                w1 = Rr[:, :, 87]
                E.ts(w1, dd, 1.0, ADD)
                E.recip(w1, w1)
                w2 = Rr[:, :, 120]
                E.tt(w2, dd, w1, MUL)
                E.tt(w1, w1, ptop, MUL)
                E.tt(w2, w2, ptop, MUL)
                Gv = Gt[:, lb0:lb0 + nbt, :]
                E.tt(lm2, o2, bcl(w2, 32), MUL)
                E.tt(Gv, o1, bcl(w1, 32), MUL)
                E.tt(Gv, Gv, lm2, ADD)
            for e in range(32):
                slot = e % 2
                if e == 0:
                    ld.start(0, 0)
                    ld.flush()
                if e + 1 < 32:
                    ld.start(e + 1, 1 - slot)
                wg, wu, wd = wgs[slot], wus[slot], wds[slot]
                for tl in tiles:
                    n = len(tl) * 128
                    l0 = (tl[0] - blocks[0]) * 128
                    for m in range(4):
                        pg, pu = (psA, psB) if m % 2 == 0 else (psC, psD)
                        for k in range(8):
                            E.mm(pg[:, 0:n], wg[:, k, m * 128:(m + 1) * 128], h2[:, k, l0:l0 + n], start=(k == 0), stop=(k == 7))
                        for k in range(8):
                            E.mm(pu[:, 0:n], wu[:, k, m * 128:(m + 1) * 128], h2[:, k, l0:l0 + n], start=(k == 0), stop=(k == 7))
                        s_ = sg[m % 2]
                        E.act(s_[:, 0:n], pg[:, 0:n], AF.Silu)
                        E.tt(he[:, m, 0:n], s_[:, 0:n], pu[:, 0:n], MUL)
                        if e + 1 < 32:
                            ld.step()
                    for bi, b in enumerate(tl):
                        lb = b - blocks[0]
                        for half in range(2):
                            po = (psE, psF, psG, psT)[(bi * 2 + half) % 4]
                            for k in range(4):
                                E.mm(po[:], he[:, k, bi * 128:(bi + 1) * 128], wd[:, k, half * 512:(half + 1) * 512],
                                     start=(k == 0), stop=(k == 3))
                            a_ = acc[:, lb, half * 512:(half + 1) * 512]
                            if e == 0:
                                E.ts(a_, po[:], Gt[:, lb, e:e + 1], MUL)
                            else:
                                E.stt(a_, po[:], Gt[:, lb, e:e + 1], a_, MUL, ADD)
                if e + 1 < 32:
                    ld.flush()
            for tl in tiles:
                n = len(tl) * 128
                c0 = tl[0] * 128
                E.dma(xt[:, :, 0:n], src[:, c0:c0 + n].rearrange("(c p) t -> p c t", p=128))
                for m in range(8):
                    pt = (psA, psB)[m % 2]
                    for bi, b in enumerate(tl):
                        lb = b - blocks[0]
                        E.tr(pt[:, bi * 128:(bi + 1) * 128], acc[:, lb, m * 128:(m + 1) * 128], ident_f)
                    E.stt(xt[:, m, 0:n], pt[:, 0:n], G2[:, m:m + 1], xt[:, m, 0:n], MUL, ADD)
                if final:
                    for c in range(8):
                        E.act(sq[:, 0:n], xt[:, c, 0:n], AF.Square)
                        E.mm(psD[:, 0:n], ones_f[:], sq[:, 0:n], start=(c == 0), stop=(c == 7))
                    rsqrt_act(E, rstd[:, 0:n], psD[:, 0:n], 1.0 / D, EPS)
                    for c in range(8):
                        E.stt(xt[:, c, 0:n], xt[:, c, 0:n], nw[:, 4, c:c + 1], rstd[:, 0:n], MUL, MUL)
                E.dma(dst[:, c0:c0 + n].rearrange("(c p) t -> p c t", p=128), xt[:, :, 0:n], final=final)


def phase_pool(ctx):
    from contextlib import ExitStack
    nc, p, E, cfg = ctx['nc'], ctx['p'], ctx['E'], ctx['cfg']
    ps = ctx['ps']
    psA, psD = ps['A'], ps['D']
    AB, cmask, ones_f = ctx['AB'], ctx['cmask'], ctx['ones_f']
    A1, B1, G1 = AB[:, 1, 0, :], AB[:, 1, 1, :], AB[:, 1, 2, :]
    x2T, x3T = ctx['x2T'], ctx['x3T']
    wins = (2, 4, 8, 16)
    with ExitStack() as es:
        def sb(name, shape, dt):
            return es.enter_context(nc.sbuf_tensor("s_pl_" + name, list(shape), dt))

        pw = sb("pw", [128, 4, 2, 256], BF16)
        for g in range(4):
            E.dma(pw[:, g, :, :], ctx['pool_w'][g].rearrange("(k p) n -> p k n", p=128), q='pool')
        psc = sb("psc", [128, 8], F32)
        E.dma(psc[:], ctx['pscale_d'])
        gps = sb("gps", [128, 8], F32)
        E.tt(gps[:], psc[:], G1, MUL)
        pfix = sb("pfix", [128, 8, 16], F32)
        E.dma(pfix[:], ctx['poolfix_d'])
        W = 528
        xt = sb("xt", [128, 8, W], F32)
        h = sb("h", [128, 8, W], F32)
        sa = sb("sa", [128, 2, W], F32)
        sbb = sb("sbb", [128, 2, W], F32)
        sq = sb("sq", [128, W], F32)
        rstd = sb("rstd", [128, W], F32)
        pl = sb("pl", [128, 8, 512], BF16)
        tf = sb("tf", [128, 2, 16], F32)
        ntile = cfg.NOWN // 512 if cfg.NOWN >= 512 else 1
        TW = min(512, cfg.NOWN)
        for ti in range(ntile):
            c0 = 128 + ti * TW - 16
            n = TW + 16
            E.dma(xt[:, :, 0:n], x2T[:, c0:c0 + n].rearrange("(c p) t -> p c t", p=128))
            for (a, bnd) in ((0, 16), (16, n)):
                for c in range(8):
                    E.act(sq[:, a:bnd], xt[:, c, a:bnd], AF.Square)
                    E.mm(psD[:, 0:bnd - a], ones_f[:], sq[:, a:bnd], start=(c == 0), stop=(c == 7))
                rsqrt_act(E, rstd[:, a:bnd], psD[:, 0:bnd - a], 1.0 / D, EPS)
            for c in range(8):
                E.tt(h[:, c, 0:n], xt[:, c, 0:n], rstd[:, 0:n], MUL)
                E.act(h[:, c, 0:n], h[:, c, 0:n], AF.Identity, bias=B1[:, c:c + 1], scale=A1[:, c:c + 1])
            if ti == 0:
                E.ts(h[:, :, 0:16], h[:, :, 0:16], cmask[:, 1:2], MUL)
            for g in range(4):
                cc = slice(2 * g, 2 * g + 2)
                cur = h[:, cc, :]
                bufs = [sa, sbb]
                step, bi_, vs = 1, 0, 0
                while step < wins[g]:
                    nxt = bufs[bi_ % 2]; bi_ += 1
                    E.tt(nxt[:, :, vs + step:n], cur[:, :, vs + step:n], cur[:, :, vs:n - step], ADD)
                    cur = nxt[:, :, :]
                    vs += step
                    step *= 2
                E.stt(pl[:, cc, 0:TW], cur[:, :, 16:n], 1.0 / wins[g], h[:, cc, 16:n], MUL, SUB)
                if ti == 0:
                    E.tt(tf[:], cur[:, :, 16:32], pfix[:, cc, :], MUL)
                    E.tt(pl[:, cc, 0:16], tf[:], h[:, cc, 16:32], SUB)
            for m in range(8):
                g = m // 2
                for kk in range(2):
                    E.mm(psA[:, 0:TW], pw[:, g, kk, (m % 2) * 128:(m % 2 + 1) * 128], pl[:, 2 * g + kk, 0:TW],
                         start=(kk == 0), stop=(kk == 1))
                E.stt(xt[:, m, 16:n], psA[:, 0:TW], gps[:, m:m + 1], xt[:, m, 16:n], MUL, ADD)
            E.dma(x3T[:, ti * TW:ti * TW + TW].rearrange("(c p) t -> p c t", p=128), xt[:, :, 16:n])


def kernel(**inputs):
    inp = {k: np.asarray(v) for k, v in inputs.items()}
    x = inp['x']
    B, L, _ = x.shape
    cfg = Cfg(L)
    nc, names = build(cfg)
    maps = prep_inputs(inp, cfg)
    maps = [{k: m[k] for k in names} for m in maps]
    res = run_bass_kernel_spmd(nc, maps, core_ids=list(range(8)))
    out = np.empty((B, L, D), np.float32)
    for core in range(8):
        b, s = core // 4, core % 4
        out[b, s * cfg.NOWN:(s + 1) * cfg.NOWN, :] = np.asarray(res.results[core]['outT']).T
    return out
```
